# Optimizing a Trainium2 kernel written in Bass

```python
import jax, jax.numpy as jnp
from jax import lax
import numpy as np

D_MODEL = 2048
BATCH = 4
SEQ = 2048
DEPTH = 4

GRID_W = 64
CTX_LEN = 256
HEAD_DIM = 128
BLOCK_Q = 128
ROPE_BASE = 10000.0
RMS_EPS = 1e-6
NEG_INF = -1e30

A_HEADS = 8
A_KV_HEADS = 2
WINDOW = 128

B_HEADS = 8
MLA_Q_RANK = 512
MLA_KV_RANK = 256
MLA_NOPE_DIM = 128
MLA_ROPE_DIM = 64
MLA_V_DIM = 128

C_HEADS = 8
C_KV_HEADS = 2

D_HEADS = 8
NB_ROWS = 8
NB_COLS = 16

N_EXPERTS = 16
CAPACITY_FACTOR = 2
D_EXPERT = 1024

A_Q = A_HEADS * HEAD_DIM
A_KV = A_KV_HEADS * HEAD_DIM
AB_SPLITS = (A_Q, A_Q + A_KV, A_Q + 2 * A_KV, A_Q + 2 * A_KV + MLA_Q_RANK, A_Q + 2 * A_KV + MLA_Q_RANK + MLA_KV_RANK)
AB_IN = A_Q + 2 * A_KV + MLA_Q_RANK + MLA_KV_RANK + MLA_ROPE_DIM
C_Q = C_HEADS * HEAD_DIM
C_KV = C_KV_HEADS * HEAD_DIM
D_QKV = D_HEADS * HEAD_DIM
CD_SPLITS = (C_Q, C_Q + C_KV, C_Q + 2 * C_KV, C_Q + 2 * C_KV + D_QKV, C_Q + 2 * C_KV + 2 * D_QKV)
CD_IN = C_Q + 2 * C_KV + 3 * D_QKV
AB_OUT = A_HEADS * HEAD_DIM + B_HEADS * MLA_V_DIM
CD_OUT = C_HEADS * HEAD_DIM + D_HEADS * HEAD_DIM

kernel_name = 'hybrid_flow_backbone'


def rmsnorm(x, g):
    x32 = x.astype(jnp.float32)
    y = x32 * lax.rsqrt(jnp.mean(x32 * x32, axis=-1, keepdims=True) + RMS_EPS)
    return (y * g.astype(jnp.float32)).astype(x.dtype)


def rope_1d(x, pos):
    d = x.shape[-1]
    inv = ROPE_BASE ** (-jnp.arange(0, d, 2, dtype=jnp.float32) / d)
    ang = pos.astype(jnp.float32)[:, None] * inv[None, :]
    cos = jnp.cos(ang)[:, None, :].astype(x.dtype)
    sin = jnp.sin(ang)[:, None, :].astype(x.dtype)
    x1, x2 = x[..., : d // 2], x[..., d // 2:]
    return jnp.concatenate([x1 * cos - x2 * sin, x2 * cos + x1 * sin], axis=-1)


def rope_2d(x, rows, cols):
    half = x.shape[-1] // 2
    return jnp.concatenate([rope_1d(x[..., :half], rows), rope_1d(x[..., half:], cols)], axis=-1)


def ctx_attend(q, k, v, scale, sink=None):
    bsz, t = q.shape[:2]
    s = jnp.einsum('btkgd,bjkd->bkgtj', q, k).astype(jnp.float32) * scale
    if sink is None:
        p = jax.nn.softmax(s, axis=-1)
    else:
        kh, g = q.shape[2], q.shape[3]
        sk = jnp.broadcast_to(sink.astype(jnp.float32).reshape(kh, g, 1, 1), s.shape[:-1] + (1,))
        p = jax.nn.softmax(jnp.concatenate([s, sk], axis=-1), axis=-1)[..., :-1]
    out = jnp.einsum('bkgtj,bjkv->btkgv', p.astype(v.dtype), v)
    return out.reshape(bsz, t, -1)


def window_attend(q, k, v, kc, vc, sink, scale):
    bsz, s_len, kh, g, d = q.shape
    nb = s_len // BLOCK_Q
    span = BLOCK_Q + 2 * WINDOW
    kp = jnp.pad(k, ((0, 0), (WINDOW, WINDOW), (0, 0), (0, 0)))
    vp = jnp.pad(v, ((0, 0), (WINDOW, WINDOW), (0, 0), (0, 0)))
    idx = jnp.arange(nb)[:, None] * BLOCK_Q + jnp.arange(span)[None, :]
    kb = kp[:, idx]
    vb = vp[:, idx]
    qpos = jnp.arange(nb)[:, None] * BLOCK_Q + jnp.arange(BLOCK_Q)[None, :]
    kpos = idx - WINDOW
    valid = ((jnp.abs(qpos[:, :, None] - kpos[:, None, :]) <= WINDOW)
             & (kpos[:, None, :] >= 0) & (kpos[:, None, :] < s_len))
    qb = q.reshape(bsz, nb, BLOCK_Q, kh, g, d)
    s_loc = jnp.einsum('bnqkgd,bnjkd->bnkgqj', qb, kb).astype(jnp.float32) * scale
    s_loc = jnp.where(valid[None, :, None, None], s_loc, NEG_INF)
    s_ctx = jnp.einsum('bnqkgd,bjkd->bnkgqj', qb, kc).astype(jnp.float32) * scale
    sk = jnp.broadcast_to(sink.astype(jnp.float32).reshape(kh, g, 1, 1), s_loc.shape[:-1] + (1,))
    p = jax.nn.softmax(jnp.concatenate([s_loc, s_ctx, sk], axis=-1), axis=-1)[..., :-1].astype(v.dtype)
    out = (jnp.einsum('bnkgqj,bnjkv->bnqkgv', p[..., :span], vb)
           + jnp.einsum('bnkgqj,bjkv->bnqkgv', p[..., span:], vc))
    return out.reshape(bsz, s_len, -1)


def dense_attend(q, k, v, kc, vc, scale):
    bsz, s_len, kh, g, d = q.shape
    nb = s_len // BLOCK_Q
    k_all = jnp.concatenate([k, kc], axis=1)
    v_all = jnp.concatenate([v, vc], axis=1)
    qb = jnp.moveaxis(q.reshape(bsz, nb, BLOCK_Q, kh, g, d), 1, 0)

    def one_block(qi):
        s = jnp.einsum('bqkgd,bjkd->bkgqj', qi, k_all).astype(jnp.float32) * scale
        p = jax.nn.softmax(s, axis=-1).astype(v_all.dtype)
        return jnp.einsum('bkgqj,bjkv->bqkgv', p, v_all)

    out = lax.map(one_block, qb)
    return jnp.moveaxis(out, 0, 1).reshape(bsz, s_len, -1)


def neighbourhood_attend(q, k, v, kc, vc, rpb, scale):
    bsz, s_len, h, d = q.shape
    rows_n = s_len // GRID_W
    kr = min(NB_ROWS, rows_n)
    qg = jnp.moveaxis(q.reshape(bsz, rows_n, GRID_W, h, d), 1, 0)
    kg = k.reshape(bsz, rows_n, GRID_W, h, d)
    vg = v.reshape(bsz, rows_n, GRID_W, h, d)
    r_idx = jnp.arange(rows_n)
    row_start = jnp.clip(r_idx - kr // 2, 0, rows_n - kr)
    col = jnp.arange(GRID_W)
    col_start = jnp.clip(col - NB_COLS // 2, 0, GRID_W - NB_COLS)
    key_row_off = jnp.repeat(jnp.arange(kr), GRID_W)
    key_col = jnp.tile(col, kr)
    col_mask = ((key_col[None, :] >= col_start[:, None])
                & (key_col[None, :] < col_start[:, None] + NB_COLS))
    dc = jnp.clip(key_col[None, :] - col[:, None] + NB_COLS - 1, 0, 2 * NB_COLS - 2)
    n_loc = kr * GRID_W

    def one_row(args):
        qr, r, rs = args
        kband = lax.dynamic_slice_in_dim(kg, rs, kr, axis=1).reshape(bsz, n_loc, h, d)
        vband = lax.dynamic_slice_in_dim(vg, rs, kr, axis=1).reshape(bsz, n_loc, h, d)
        dr = rs + key_row_off - r + NB_ROWS - 1
        bias = rpb[:, dr[None, :], dc].astype(jnp.float32)
        s_loc = jnp.einsum('bqhd,bjhd->bhqj', qr, kband).astype(jnp.float32) * scale + bias[None]
        s_loc = jnp.where(col_mask[None, None], s_loc, NEG_INF)
        s_ctx = jnp.einsum('bqhd,bjhd->bhqj', qr, kc).astype(jnp.float32) * scale
        p = jax.nn.softmax(jnp.concatenate([s_loc, s_ctx], axis=-1), axis=-1).astype(v.dtype)
        return (jnp.einsum('bhqj,bjhv->bqhv', p[..., :n_loc], vband)
                + jnp.einsum('bhqj,bjhv->bqhv', p[..., n_loc:], vc))

    out = lax.map(one_row, (qg, r_idx, row_start))
    return jnp.moveaxis(out, 0, 1).reshape(bsz, s_len, h * d)


def project_ab(h, rows, cols, rotate, w_in, q_norm, w_uq, kv_norm, w_ukv):
    bsz, t, _ = h.shape
    aq, ak, av, cq, ckv, k_rope = jnp.split(h @ w_in, AB_SPLITS, axis=-1)
    aq = aq.reshape(bsz, t, A_HEADS, HEAD_DIM)
    ak = ak.reshape(bsz, t, A_KV_HEADS, HEAD_DIM)
    av = av.reshape(bsz, t, A_KV_HEADS, HEAD_DIM)
    bq = (rmsnorm(cq, q_norm) @ w_uq).reshape(bsz, t, B_HEADS, MLA_NOPE_DIM + MLA_ROPE_DIM)
    bkv = (rmsnorm(ckv, kv_norm) @ w_ukv).reshape(bsz, t, B_HEADS, MLA_NOPE_DIM + MLA_V_DIM)
    bq_nope, bq_rope = bq[..., :MLA_NOPE_DIM], bq[..., MLA_NOPE_DIM:]
    bk_nope, bv = bkv[..., :MLA_NOPE_DIM], bkv[..., MLA_NOPE_DIM:]
    k_rope = k_rope[:, :, None, :]
    if rotate:
        aq = rope_2d(aq, rows, cols)
        ak = rope_2d(ak, rows, cols)
        bq_rope = rope_2d(bq_rope, rows, cols)
        k_rope = rope_2d(k_rope, rows, cols)
    aq = aq.reshape(bsz, t, A_KV_HEADS, A_HEADS // A_KV_HEADS, HEAD_DIM)
    bq = jnp.concatenate([bq_nope, bq_rope], axis=-1)[:, :, :, None, :]
    bk = jnp.concatenate([bk_nope, jnp.broadcast_to(k_rope, (bsz, t, B_HEADS, MLA_ROPE_DIM))], axis=-1)
    return aq, ak, av, bq, bk, bv


def mixer_ab(hc, hl, rows, cols, need_ctx, w_in, sink, q_norm, w_uq, kv_norm, w_ukv, w_out):
    weights = (w_in, q_norm, w_uq, kv_norm, w_ukv)
    caq, cak, cav, cbq, cbk, cbv = project_ab(hc, None, None, False, *weights)
    laq, lak, lav, lbq, lbk, lbv = project_ab(hl, rows, cols, True, *weights)
    scale_a = HEAD_DIM ** -0.5
    scale_b = (MLA_NOPE_DIM + MLA_ROPE_DIM) ** -0.5
    y_lat = jnp.concatenate([window_attend(laq, lak, lav, cak, cav, sink, scale_a),
                             dense_attend(lbq, lbk, lbv, cbk, cbv, scale_b)], axis=-1) @ w_out
    y_ctx = None
    if need_ctx:
        y_ctx = jnp.concatenate([ctx_attend(caq, cak, cav, scale_a, sink),
                                 ctx_attend(cbq, cbk, cbv, scale_b)], axis=-1) @ w_out
    return y_ctx, y_lat


def project_cd(h, rows, cols, rotate, w_in, q_norm, k_norm):
    bsz, t, _ = h.shape
    cq, ck, cv, dq, dk, dv = jnp.split(h @ w_in, CD_SPLITS, axis=-1)
    cq = rmsnorm(cq.reshape(bsz, t, C_HEADS, HEAD_DIM), q_norm)
    ck = rmsnorm(ck.reshape(bsz, t, C_KV_HEADS, HEAD_DIM), k_norm)
    cv = cv.reshape(bsz, t, C_KV_HEADS, HEAD_DIM)
    if rotate:
        cq = rope_2d(cq, rows, cols)
        ck = rope_2d(ck, rows, cols)
    cq = cq.reshape(bsz, t, C_KV_HEADS, C_HEADS // C_KV_HEADS, HEAD_DIM)
    dq = dq.reshape(bsz, t, D_HEADS, HEAD_DIM)
    dk = dk.reshape(bsz, t, D_HEADS, HEAD_DIM)
    dv = dv.reshape(bsz, t, D_HEADS, HEAD_DIM)
    return cq, ck, cv, dq, dk, dv


def mixer_cd(hc, hl, rows, cols, need_ctx, w_in, q_norm, k_norm, rpb, w_out):
    ccq, cck, ccv, cdq, cdk, cdv = project_cd(hc, None, None, False, w_in, q_norm, k_norm)
    lcq, lck, lcv, ldq, ldk, ldv = project_cd(hl, rows, cols, True, w_in, q_norm, k_norm)
    scale = HEAD_DIM ** -0.5
    y_lat = jnp.concatenate([dense_attend(lcq, lck, lcv, cck, ccv, scale),
                             neighbourhood_attend(ldq, ldk, ldv, cdk, cdv, rpb, scale)], axis=-1) @ w_out
    y_ctx = None
    if need_ctx:
        y_ctx = jnp.concatenate([ctx_attend(ccq, cck, ccv, scale),
                                 ctx_attend(cdq[:, :, :, None, :], cdk, cdv, scale)], axis=-1) @ w_out
    return y_ctx, y_lat


def ec_moe(h, w_router, w_gate, w_up, w_down):
    bsz, t, d = h.shape
    cap = CAPACITY_FACTOR * t // N_EXPERTS
    aff = jax.nn.softmax(jnp.einsum('btd,de->bte', h, w_router).astype(jnp.float32), axis=-1)
    g, idx = lax.top_k(jnp.swapaxes(aff, 1, 2), cap)
    xin = jax.vmap(lambda hb, ib: hb[ib])(h, idx)
    hid = (jax.nn.silu(jnp.einsum('becd,edf->becf', xin, w_gate))
           * jnp.einsum('becd,edf->becf', xin, w_up))
    out = jnp.einsum('becf,efd->becd', hid, w_down) * g[..., None].astype(h.dtype)
    return jax.vmap(lambda ob, ib: jnp.zeros((t, d), ob.dtype).at[ib.reshape(-1)].add(ob.reshape(-1, d)))(out, idx)


def setup_inputs(seed: int = 0) -> dict:
    key = jax.random.key(seed)
    ks = jax.random.split(key, 25)
    n_ab = (DEPTH + 1) // 2
    n_cd = DEPTH // 2

    def nrm(k, shape, scale):
        return jax.random.normal(k, shape, jnp.float32) * scale

    def gain(k, shape):
        return 1.0 + 0.1 * jax.random.normal(k, shape, jnp.float32)

    return {
        'x': nrm(ks[0], (BATCH, SEQ, D_MODEL), 1.0),
        'c': nrm(ks[1], (BATCH, D_MODEL), 1.0),
        'ctx': nrm(ks[2], (BATCH, CTX_LEN, D_MODEL), 1.0),
        'c_ctx': nrm(ks[3], (D_MODEL,), 1.0),
        'w_ada': nrm(ks[4], (DEPTH, D_MODEL, 6 * D_MODEL), 0.5 * D_MODEL ** -0.5),
        'b_ada': nrm(ks[5], (DEPTH, 6 * D_MODEL), 0.02),
        'norm1': gain(ks[6], (DEPTH, D_MODEL)),
        'norm2': gain(ks[7], (DEPTH, D_MODEL)),
        'ab_w_in': nrm(ks[8], (n_ab, D_MODEL, AB_IN), D_MODEL ** -0.5),
        'ab_sink': nrm(ks[9], (n_ab, A_HEADS), 0.5),
        'ab_q_norm': gain(ks[10], (n_ab, MLA_Q_RANK)),
        'ab_w_uq': nrm(ks[11], (n_ab, MLA_Q_RANK, B_HEADS * (MLA_NOPE_DIM + MLA_ROPE_DIM)), MLA_Q_RANK ** -0.5),
        'ab_kv_norm': gain(ks[12], (n_ab, MLA_KV_RANK)),
        'ab_w_ukv': nrm(ks[13], (n_ab, MLA_KV_RANK, B_HEADS * (MLA_NOPE_DIM + MLA_V_DIM)), MLA_KV_RANK ** -0.5),
        'ab_w_out': nrm(ks[14], (n_ab, AB_OUT, D_MODEL), AB_OUT ** -0.5),
        'cd_w_in': nrm(ks[15], (n_cd, D_MODEL, CD_IN), D_MODEL ** -0.5),
        'cd_q_norm': gain(ks[16], (n_cd, HEAD_DIM)),
        'cd_k_norm': gain(ks[17], (n_cd, HEAD_DIM)),
        'cd_rpb': nrm(ks[18], (n_cd, D_HEADS, 2 * NB_ROWS - 1, 2 * NB_COLS - 1), 0.1),
        'cd_w_out': nrm(ks[19], (n_cd, CD_OUT, D_MODEL), CD_OUT ** -0.5),
        'w_router': nrm(ks[20], (DEPTH, D_MODEL, N_EXPERTS), D_MODEL ** -0.5),
        'w_gate': nrm(ks[21], (DEPTH, N_EXPERTS, D_MODEL, D_EXPERT), D_MODEL ** -0.5),
        'w_up': nrm(ks[22], (DEPTH, N_EXPERTS, D_MODEL, D_EXPERT), D_MODEL ** -0.5),
        'w_down': nrm(ks[23], (DEPTH, N_EXPERTS, D_EXPERT, D_MODEL), D_EXPERT ** -0.5),
        'final_norm': gain(ks[24], (D_MODEL,)),
    }


def reference(x, c, ctx, c_ctx, w_ada, b_ada, norm1, norm2, ab_w_in, ab_sink, ab_q_norm, ab_w_uq,
              ab_kv_norm, ab_w_ukv, ab_w_out, cd_w_in, cd_q_norm, cd_k_norm, cd_rpb, cd_w_out,
              w_router, w_gate, w_up, w_down, final_norm):
    s_len = x.shape[1]
    t = jnp.arange(s_len)
    rows, cols = t // GRID_W, t % GRID_W
    xl, xc = x, ctx
    for l in range(DEPTH):
        need_ctx = l < DEPTH - 1
        mod_l = (jax.nn.silu(c) @ w_ada[l] + b_ada[l])[:, None, :]
        mod_c = (jax.nn.silu(c_ctx) @ w_ada[l] + b_ada[l])[None, None, :]
        sh1, sc1, g1, sh2, sc2, g2 = jnp.split(mod_l, 6, axis=-1)
        csh1, csc1, cg1, csh2, csc2, cg2 = jnp.split(mod_c, 6, axis=-1)
        hl = rmsnorm(xl, norm1[l]) * (1.0 + sc1) + sh1
        hc = rmsnorm(xc, norm1[l]) * (1.0 + csc1) + csh1
        i = l // 2
        if l % 2 == 0:
            y_ctx, y_lat = mixer_ab(hc, hl, rows, cols, need_ctx, ab_w_in[i], ab_sink[i], ab_q_norm[i],
                                    ab_w_uq[i], ab_kv_norm[i], ab_w_ukv[i], ab_w_out[i])
        else:
            y_ctx, y_lat = mixer_cd(hc, hl, rows, cols, need_ctx, cd_w_in[i], cd_q_norm[i], cd_k_norm[i],
                                    cd_rpb[i], cd_w_out[i])
        xl = xl + g1 * y_lat
        hl = rmsnorm(xl, norm2[l]) * (1.0 + sc2) + sh2
        xl = xl + g2 * ec_moe(hl, w_router[l], w_gate[l], w_up[l], w_down[l])
        if need_ctx:
            xc = xc + cg1 * y_ctx
            hc = rmsnorm(xc, norm2[l]) * (1.0 + csc2) + csh2
            xc = xc + cg2 * ec_moe(hc, w_router[l], w_gate[l], w_up[l], w_down[l])
    return rmsnorm(xl, final_norm)
```

```python
import math
from contextlib import ExitStack

import numpy as np
import ml_dtypes
import concourse.bass as bass
import concourse.mybir as mybir
from concourse.bass_utils import run_bass_kernel_spmd

F32 = mybir.dt.float32
BF16 = mybir.dt.bfloat16
I32 = mybir.dt.int32
U32 = mybir.dt.uint32
AF = mybir.ActivationFunctionType
ALU = mybir.AluOpType

D = 2048
S = 2048
LC = 256
T = S + LC
NT = T // 128
DEPTH = 4
EPS = 1e-6
NE = 16
CAP_L = 256
CAP_C = 32
CAPT = CAP_L + CAP_C
DE = 1024
TBLK = [(0, 512), (512, 512), (1024, 512), (1536, 512), (2048, 256)]
N_CORES = 4
P2_LIMIT = 6
DBG_NOROPE = False
ROPE_STEPS = 9
ATT_LIMIT = 10 ** 9
ATT_COUNT = [0]
ATT_STEPS = 9
MOE_STEPS = 9
ROPE_ADD_ENG = 'pool'


class Res:
    __slots__ = ("name", "w", "r")

    def __init__(self, name=""):
        self.name = name
        self.w = {}
        self.r = {}


class DSem:
    def __init__(self, nc, name):
        self.sem = nc.alloc_semaphore(name)
        self.cum = 0


class Prog:
    ENG = ["sp", "act", "pool", "dve", "pe"]

    def __init__(self, nc):
        self.nc = nc
        self.ops = {e: [] for e in self.ENG}
        self.cnt = {e: 0 for e in self.ENG}
        self.esem = {e: nc.alloc_semaphore("es_" + e) for e in self.ENG}
        self.seen = {e: {} for e in self.ENG}
        self._dsems = {}
        self.ninst = 0

    def dsem(self, name):
        if name not in self._dsems:
            self._dsems[name] = DSem(self.nc, "ds_" + name)
        return self._dsems[name]

    def _collect(self, eng, reads, writes, awrites=(), extra=()):
        waits = {}
        own = self.esem[eng]

        def need(sem, val):
            if sem is own and eng == "pe":
                return
            if val > self.seen[eng].get(sem, 0) and val > waits.get(sem, 0):
                waits[sem] = val

        for r in reads:
            for s, v in r.w.items():
                need(s, v)
        for w in writes:
            for s, v in w.w.items():
                need(s, v)
            for s, v in w.r.items():
                need(s, v)
        for w in awrites:
            for s, v in w.r.items():
                need(s, v)
        for s, v in extra:
            need(s, v)
        for s, v in waits.items():
            self.seen[eng][s] = v
        return list(waits.items())

    def _mark(self, tok, reads, writes, awrites=()):
        s, v = tok
        for r in reads:
            if v > r.r.get(s, 0):
                r.r[s] = v
        for w in writes:
            w.w = {s: v}
            w.r = {}
        for w in awrites:
            if v > w.w.get(s, 0):
                w.w[s] = v

    def op(self, eng, fn, reads=(), writes=(), awrites=()):
        waits = self._collect(eng, reads, writes, awrites)
        self.cnt[eng] += 1
        tok = (self.esem[eng], self.cnt[eng])
        self.ops[eng].append((waits, fn, (self.esem[eng], 1)))
        self._mark(tok, reads, writes, awrites)
        return tok

    def dma(self, q, fn, dsem, n=1, reads=(), writes=(), awrites=()):
        if isinstance(dsem, str):
            dsem = self.dsem(dsem)
        waits = self._collect(q, reads, writes, awrites, extra=[(dsem.sem, dsem.cum)] if dsem.cum else [])
        dsem.cum += 16 * n
        tok = (dsem.sem, dsem.cum)
        self.ops[q].append((waits, fn, (dsem.sem, 16)))
        self._mark(tok, reads, writes, awrites)
        return tok

    def barrier(self):
        toks = [(self.esem[e], self.cnt[e]) for e in self.ENG if self.cnt[e] > 0]
        toks += [(d.sem, d.cum) for d in self._dsems.values() if d.cum]
        for e in self.ENG:
            waits = self._collect(e, (), (), extra=toks)
            if waits:
                self.ops[e].append((waits, None, None))

    def emit(self, final_tokens=()):
        nc = self.nc
        fw = {}
        for s, v in final_tokens:
            fw[s] = max(fw.get(s, 0), v)

        def run(name, e):
            for waits, fn, inc in self.ops[name]:
                for s, v in waits:
                    e.wait_ge(s, v)
                if fn is None:
                    continue
                r = fn(e)
                sem, k = inc
                if isinstance(r, (list, tuple)):
                    for ins in r:
                        ins.then_inc(sem, k)
                else:
                    r.then_inc(sem, k)
            if name == "sp":
                for s, v in fw.items():
                    e.wait_ge(s, v)

        with nc.Block() as block:
            @block.sync
            def _(e):
                run("sp", e)

            @block.scalar
            def _(e):
                run("act", e)

            @block.gpsimd
            def _(e):
                run("pool", e)

            @block.vector
            def _(e):
                run("dve", e)

            @block.tensor
            def _(e):
                run("pe", e)


class Slot:
    __slots__ = ("t", "r", "ds")

    def __init__(self, t, r, ds):
        self.t = t
        self.r = r
        self.ds = ds


class Ring:
    def __init__(self, alloc, name, shape, dtype, bufs):
        self.s = [Slot(alloc(f"{name}{i}", shape, dtype), Res(f"{name}{i}"), f"{name}{i}") for i in range(bufs)]
        self.i = 0

    def next(self):
        s = self.s[self.i % len(self.s)]
        self.i += 1
        return s


def bcast_rows(ap, n):
    return bass.AP(ap.tensor, ap.offset, [[0, n]] + [list(x) for x in ap.ap[1:]])


def _rope_tables():
    t = np.arange(S)
    rows, cols = t // 64, t % 64

    def tab(dh):
        half = dh // 2
        nf = half // 2
        inv = 10000.0 ** (-np.arange(0, half, 2, dtype=np.float32) / half)
        C = np.ones((dh, T), np.float32)
        Sg = np.zeros((dh, T), np.float32)
        for f in range(dh):
            pos = rows if f < half else cols
            ff = f % half
            fi = ff % nf
            ang = pos.astype(np.float32) * inv[fi]
            C[f, :S] = np.cos(ang)
            Sg[f, :S] = (-np.sin(ang)) if ff < nf else np.sin(ang)
        Pm = np.zeros((dh, dh), np.float32)
        for f in range(dh):
            ff = f % half
            partner = f + nf if ff < nf else f - nf
            Pm[partner, f] = 1.0
        return C, Sg, Pm

    return tab(128), tab(64)


def _band_mask():
    j = np.arange(128)[:, None]
    i = np.arange(384)[None, :]
    return (np.abs(i - 128 - j) <= 128).astype(np.float32)


def _nbr_plan():
    rows_n = 32
    kc = np.arange(64)[:, None]
    qc = np.arange(64)[None, :]
    cs = np.clip(qc - 8, 0, 48)
    colmask = ((kc >= cs) & (kc < cs + 16)).astype(np.float32)
    pats = {}
    plan = []
    for qt in range(16):
        rs = [int(np.clip(r - 4, 0, rows_n - 8)) for r in (2 * qt, 2 * qt + 1)]
        k_lo = min(rs) // 2
        k_hi = (max(rs) + 7) // 2
        ent = []
        for kt in range(k_lo, k_hi + 1):
            blk = []
            for kr in range(2):
                for qr in range(2):
                    krow = 2 * kt + kr
                    blk.append(1 if rs[qr] <= krow < rs[qr] + 8 else 0)
            key = tuple(blk)
            if sum(key) == 0:
                continue
            if key not in pats:
                m = np.zeros((128, 128), np.float32)
                for kr in range(2):
                    for qr in range(2):
                        if key[kr * 2 + qr]:
                            m[kr * 64:(kr + 1) * 64, qr * 64:(qr + 1) * 64] = colmask
                pats[key] = (len(pats), m)
            ent.append((kt, kt - qt, pats[key][0]))
        plan.append(ent)
    masks = np.stack([m for _, m in sorted(pats.values(), key=lambda x: x[0])])
    return plan, masks


ROPE128, ROPE64 = _rope_tables()
BAND = _band_mask()
NBR_PLAN, NBR_MASKS = _nbr_plan()
NPAT = NBR_MASKS.shape[0]


def _bf(a):
    return np.ascontiguousarray(a).astype(ml_dtypes.bfloat16)


INPUT_SHAPES = {
    "x": ([S, D], "f"), "ctx": ([LC, D], "f"), "cvec": ([2, D], "f"),
    "w_ada": ([DEPTH, D, 6 * D], "f"), "b_ada": ([DEPTH, 6 * D], "f"),
    "norm1": ([DEPTH, D], "f"), "norm2": ([DEPTH, D], "f"),
    "ab_w_in": ([2, D, 2368], "f"), "ab_sink": ([2, 8], "f"), "ab_q_norm": ([2, 512], "f"),
    "ab_w_uq": ([2, 512, 1536], "f"), "ab_kv_norm": ([2, 256], "f"), "ab_w_ukv": ([2, 256, 2048], "f"),
    "ab_w_out": ([2, D, D], "f"), "cd_w_in": ([2, D, 4608], "f"), "cd_q_norm": ([2, 128], "f"),
    "cd_k_norm": ([2, 128], "f"), "cd_rpb": ([2, 8, 15, 31], "f"), "cd_w_out": ([2, D, D], "f"),
    "w_router": ([DEPTH, D, NE], "f"), "w_gate": ([DEPTH, NE, D, DE], "f"), "w_up": ([DEPTH, NE, D, DE], "f"),
    "w_down": ([DEPTH, NE, DE, D], "f"), "final_norm": ([1, D], "f"),
    "c_rc128": ([128, T], "b"), "c_rs128": ([128, T], "b"), "c_rp128": ([128, 128], "b"),
    "c_rc64": ([64, T], "b"), "c_rs64": ([64, T], "b"), "c_rp64": ([64, 64], "b"),
    "c_band": ([128, 384], "b"), "c_nbrm": ([128, NPAT, 128], "b"), "c_J": ([128, 128], "f"),
}


def build(n_layers=DEPTH, debug=False, stop=None):
    nc = bass.Bass("TRN2", target_bir_lowering=False)
    p = Prog(nc)
    used = {}

    def I(name):
        if name not in used:
            shp, k = INPUT_SHAPES[name]
            used[name] = nc.dram_tensor(name, list(shp), F32 if k == "f" else BF16, kind="ExternalInput").ap()
        return used[name]

    nc.used_inputs = used

    def dscr(name, shape, dt):
        return nc.dram_tensor(name, list(shape), dt, kind="ExternalOutput" if debug else "Internal").ap()

    out = nc.dram_tensor("out", [S, D], F32, kind="ExternalOutput").ap()
    xres = dscr("xres", [T, D], F32)
    xn_d = dscr("xn_d", [T, D], BF16)
    modrow_d = dscr("modrow_d", [DEPTH, 2, 6 * D], F32)
    FM_d = dscr("FM_d", [4096, T], BF16)
    TM_d = dscr("TM_d", [T, 1280], BF16)
    rpbpad_d = dscr("rpbpad_d", [120, 160], F32)

    xres_r = [Res(f"xres{i}") for i in range(NT)]
    xn_r = Res("xn_d")
    modrow_r = Res("modrow")
    FM_r = Res("FM")
    TM_r = Res("TM")
    rpb_r = Res("rpbpad")
    out_r = Res("out")

    def A(name, shape, dt):
        return nc.alloc_sbuf_tensor(name, list(shape), dt)

    uid = [0]

    def AS(es, name, shape, dt):
        uid[0] += 1
        return es.enter_context(nc.sbuf_tensor(f"{name}_u{uid[0]}", list(shape), dt))

    def ringS(es, name, shape, dt, bufs):
        return Ring(lambda n, s, d: AS(es, n, s, d), name, shape, dt, bufs)

    ident_f = A("ident_f", [128, 128], F32)
    ident_b = A("ident_b", [128, 128], BF16)
    ones_b = A("ones_b", [128, 128], BF16)
    ones_f = A("ones_f", [1, 128], F32)
    rp128 = A("rp128", [128, 128], BF16)
    rp64 = A("rp64", [64, 64], BF16)
    cst_r = Res("consts")
    scT = A("scT", [128, 16, 2], BF16)
    ncol = A("ncol", [128, 8, 16], F32)
    small_r = Res("small")
    modcol = [A(f"modcol{l}", [128, 96, 2], F32) for l in range(DEPTH)]
    modcol_r = [Res(f"modcol{l}") for l in range(DEPTH)]
    affT = A("affT", [16, T], F32)
    affT_r = Res("affT")
    Wr = Ring(A, "W", [128, 8192], BF16, 4)

    psb = [nc.alloc_psum_tensor(f"ps{i}", [128, 512], F32) for i in range(8)]
    psr = [Res(f"ps{i}") for i in range(8)]

    class PsRing:
        def __init__(self, idxs):
            self.idxs = idxs
            self.i = 0

        def next(self):
            k = self.idxs[self.i % len(self.idxs)]
            self.i += 1
            return psb[k], psr[k]

    def dma1(out_ap, in_ap, **kw):
        return lambda e: [e.dma_start(out=out_ap, in_=in_ap, **kw)]

    NCG = dict(allow_slow_non_contiguous=True)

    def wload(src_ap, ncols, kch):
        wb = Wr.next()
        wv = wb.t[:, 0:kch * ncols].rearrange("p (c n) -> p c n", c=kch)
        p.dma("pool", dma1(wv, src_ap.rearrange("(c k) n -> k c n", k=128)), wb.ds, 1, writes=[wb.r])
        return wb, wv

    p.op("pool", lambda e: e.memset(ident_f[:], 0.0), writes=[cst_r])
    p.op("pool", lambda e: e.affine_select(out=ident_f[:], in_=ident_f[:], pattern=[[-1, 128]], compare_op=ALU.not_equal,
                                           fill=1.0, base=0, channel_multiplier=1), reads=[cst_r], writes=[cst_r])
    p.op("dve", lambda e: e.tensor_copy(out=ident_b[:], in_=ident_f[:]), reads=[cst_r], writes=[cst_r])
    p.op("dve", lambda e: e.memset(ones_b[:], 1.0), writes=[cst_r])
    p.op("dve", lambda e: e.memset(ones_f[:], 1.0), writes=[cst_r])
    for dst, src in [(rp128, "c_rp128"), (rp64, "c_rp64")]:
        p.dma("sp", dma1(dst[:], I(src)), "cst", 1, writes=[cst_r])

    with ExitStack() as es0:
        craw = AS(es0, "craw", [128, 16, 2], F32)
        for r in range(2):
            p.dma("sp", dma1(craw[:, :, r], I("cvec")[r].rearrange("(c p) -> p c", p=128), **NCG), "sm", 1, writes=[small_r])
        for l in range(DEPTH):
            p.dma("sp", dma1(ncol[:, l, :], I("norm1")[l].rearrange("(c p) -> p c", p=128), **NCG), "sm", 1, writes=[small_r])
            p.dma("sp", dma1(ncol[:, 4 + l, :], I("norm2")[l].rearrange("(c p) -> p c", p=128), **NCG), "sm", 1, writes=[small_r])
        p.op("act", lambda e: e.activation(out=scT[:], in_=craw[:], func=AF.Silu), reads=[small_r], writes=[small_r])

        rbR = ringS(es0, "rb", [2, 512], F32, 2)
        btR = ringS(es0, "bt", [2, 512], F32, 2)
        psM = PsRing([0, 1])
        psTm = PsRing([2, 3])
        for l in range(n_layers):
            for nb in range(24):
                wb, wv = wload(I("w_ada")[l, :, nb * 512:(nb + 1) * 512], 512, 16)
                bt = btR.next()
                p.dma("sp", dma1(bt.t[:], bcast_rows(I("b_ada")[l:l + 1, nb * 512:(nb + 1) * 512], 2)), bt.ds, 1, writes=[bt.r])
                ps, pr = psM.next()

                def mm(e, ps=ps, wv=wv):
                    for k in range(16):
                        r = e.matmul(ps[0:2, :], lhsT=scT[:, k, :], rhs=wv[:, k, :], start=(k == 0), stop=(k == 15))
                    return r
                p.op("pe", mm, reads=[wb.r, small_r], writes=[pr])
                rb = rbR.next()
                p.op("dve", lambda e, ps=ps, rb=rb, bt=bt: e.tensor_tensor(out=rb.t[:], in0=ps[0:2, :], in1=bt.t[:], op=ALU.add),
                     reads=[pr, bt.r], writes=[rb.r])
                p.dma("sp", dma1(modrow_d[l, :, nb * 512:(nb + 1) * 512], rb.t[:]), rb.ds, 1, reads=[rb.r], awrites=[modrow_r])
                pt, ptr = psTm.next()

                def tr(e, pt=pt, rb=rb):
                    for j in range(4):
                        r = e.transpose(out=pt[:, j * 2:(j + 1) * 2], in_=rb.t[0:2, j * 128:(j + 1) * 128], identity=ident_f[0:2, 0:2])
                    return r
                p.op("pe", tr, reads=[rb.r, cst_r], writes=[ptr])
                mcv = modcol[l][:, :, :].rearrange("p c r -> p (c r)")
                p.op("dve", lambda e, pt=pt, mcv=mcv, nb=nb: e.tensor_copy(out=mcv[:, nb * 8:(nb + 1) * 8], in_=pt[:, 0:8]),
                     reads=[ptr], writes=[modcol_r[l]])
        p.barrier()

    def finish(extra_res=()):
        toks = []
        for r in list(extra_res) + [out_r, modrow_r, FM_r, TM_r, xn_r] + xres_r:
            toks += list(r.w.items())
        p.barrier()
        p.emit(toks)
        return nc

    if stop == "mod":
        return finish()

    SQD = math.sqrt(D)

    def make_AB(es, l, which):
        Am = AS(es, f"Am{which}", [128, 16, 2], F32)
        r_ = Res(f"Am{which}")
        sc0 = 16 if which == 1 else 64
        sh0 = 0 if which == 1 else 48
        nrow = l if which == 1 else 4 + l
        p.op("dve", lambda e: e.tensor_scalar(out=Am[:], in0=modcol[l][:, sc0:sc0 + 16, :], scalar1=1.0, scalar2=SQD,
                                              op0=ALU.add, op1=ALU.mult), reads=[modcol_r[l]], writes=[r_])
        for r in range(2):
            p.op("dve", lambda e, r=r: e.tensor_tensor(out=Am[:, :, r], in0=Am[:, :, r], in1=ncol[:, nrow, :], op=ALU.mult),
                 reads=[r_, small_r], writes=[r_])
        Bm = modcol[l][:, sh0:sh0 + 16, :]
        return Am, Bm, r_

    def xsrc(l, i):
        if l == 0:
            return I("x")[i * 128:(i + 1) * 128, :] if i < 16 else I("ctx")[(i - 16) * 128:(i - 15) * 128, :]
        return xres[i * 128:(i + 1) * 128, :]

    def norm_tile(xt, ssR, junk, junk_r):
        ss = ssR.next()
        p.op("act", lambda e: e.activation(out=junk[:], in_=xt.t[:], func=AF.Square, accum_out=ss.t[:, 0:1]),
             reads=[xt.r], writes=[junk_r, ss.r])
        p.op("act", lambda e: e.activation(out=ss.t[:, 1:2], in_=ss.t[:, 0:1], func=AF.Sqrt, scale=1.0, bias=D * EPS),
             reads=[ss.r], writes=[ss.r])
        p.op("dve", lambda e: e.reciprocal(out=ss.t[:, 2:3], in_=ss.t[:, 1:2]), reads=[ss.r], writes=[ss.r])
        return ss

    def phase1(es, l, big, big_rs):
        Am, Bm, am_r = make_AB(es, l, 1)
        xtR = ringS(es, "p1x", [128, D], F32, 2)
        xnR = ringS(es, "p1n", [128, D], BF16, 5)
        junk = AS(es, "p1junk", [128, D], BF16)
        junk_r = Res("junk")
        ssR = ringS(es, "p1s", [128, 4], F32, 4)
        psT = PsRing([0, 1, 2, 3])
        ev = 0
        for (t0, tn) in TBLK:
            r = 0 if t0 < S else 1
            xns = []
            for ii in range(tn // 128):
                i = t0 // 128 + ii
                xt = xtR.next()
                p.dma("sp", dma1(xt.t[:], xsrc(l, i)), xt.ds, 1, reads=[xres_r[i]], writes=[xt.r])
                ss = norm_tile(xt, ssR, junk, junk_r)
                xn = xnR.next()
                p.op("act", lambda e, xt=xt, ss=ss, xn=xn: e.activation(out=xn.t[:], in_=xt.t[:], func=AF.Copy, scale=ss.t[:, 2:3]),
                     reads=[xt.r, ss.r], writes=[xn.r])
                xns.append(xn)
            for c in range(16):
                pt, ptr = psT.next()
                ptb = pt[:, :].bitcast(BF16)

                def tr(e, ptb=ptb, xns=xns, c=c):
                    for ii, xn in enumerate(xns):
                        r_ = e.transpose(out=ptb[:, ii * 128:(ii + 1) * 128], in_=xn.t[:, c * 128:(c + 1) * 128], identity=ident_b[:])
                    return r_
                p.op("pe", tr, reads=[x.r for x in xns] + [cst_r], writes=[ptr])
                dst = big[:, c, t0:t0 + tn]
                if ev % 2 == 0:
                    p.op("act", lambda e, ptb=ptb, dst=dst, c=c, r=r, tn=tn: e.activation(
                        out=dst, in_=ptb[:, 0:tn], func=AF.Identity, scale=Am[:, c, r:r + 1], bias=Bm[:, c, r:r + 1]),
                        reads=[ptr, am_r, modcol_r[l]], awrites=[big_rs[c]])
                else:
                    p.op("dve", lambda e, ptb=ptb, dst=dst, c=c, r=r, tn=tn: e.tensor_scalar(
                        out=dst, in0=ptb[:, 0:tn], scalar1=Am[:, c, r:r + 1], scalar2=Bm[:, c, r:r + 1], op0=ALU.mult, op1=ALU.add),
                        reads=[ptr, am_r, modcol_r[l]], awrites=[big_rs[c]])
                ev += 1

    class ProjCtx:
        pass

    def proj_setup(es):
        c = ProjCtx()
        c.qaR = ringS(es, "pqa", [128, 512], BF16, 2)
        c.t1R = ringS(es, "pt1", [128, 512], F32, 1)
        c.t2R = ringS(es, "pt2", [128, 512], F32, 1)
        c.osR = ringS(es, "pos", [128, 512], BF16, 3)
        c.sqR = ringS(es, "psq", [128, 512], BF16, 2)
        c.rbR = ringS(es, "prb", [128, 512], F32, 1)
        c.tmR = ringS(es, "ptm", [128, 512], F32, 1)
        c.rtR = {128: ringS(es, "prt", [128, 2, 512], BF16, 2), 64: ringS(es, "prt6", [64, 2, 512], BF16, 2)}
        c.rt_cur = {}
        c.psA = PsRing([0, 1, 2, 3])
        c.psP = PsRing([4, 5])
        c.psV = PsRing([6, 7])
        c.ev = 0
        return c

    def fm_matmul(c, lhs_fn, nk, rhs_fn, M, n, reads):
        ps, pr = c.psA.next()

        def f(e):
            for k in range(nk):
                r = e.matmul(ps[0:M, 0:n], lhsT=lhs_fn(k), rhs=rhs_fn(k), start=(k == 0), stop=(k == nk - 1))
            return r
        p.op("pe", f, reads=reads, writes=[pr])
        return ps, pr

    def store_fm(c, src_ap, src_reads, M, n, row0, t0, eng=None):
        o = c.osR.next()
        eng = eng or ("act" if c.ev % 2 == 0 else "dve")
        c.ev += 1
        if eng == "act":
            p.op("act", lambda e: e.activation(out=o.t[0:M, 0:n], in_=src_ap, func=AF.Copy), reads=src_reads, writes=[o.r])
        else:
            p.op(eng, lambda e: e.tensor_copy(out=o.t[0:M, 0:n], in_=src_ap), reads=src_reads, writes=[o.r])
        p.dma("sp", dma1(FM_d[row0:row0 + M, t0:t0 + n], o.t[0:M, 0:n]), o.ds, 1, reads=[o.r], awrites=[FM_r])

    def rope_tabs(c, which, t0, n):
        key = (which, t0)
        if c.rt_cur.get(which, (None, None))[0] != key:
            sl = c.rtR[which].next()
            cn, sn = ("c_rc128", "c_rs128") if which == 128 else ("c_rc64", "c_rs64")
            p.dma("sp", dma1(sl.t[:, 0, 0:n], I(cn)[:, t0:t0 + n]), sl.ds, 1, writes=[sl.r])
            p.dma("sp", dma1(sl.t[:, 1, 0:n], I(sn)[:, t0:t0 + n]), sl.ds, 1, awrites=[sl.r])
            c.rt_cur[which] = (key, sl)
        return c.rt_cur[which][1]

    def rope_store(c, src_ap, src_reads, M, n, t0, which, row0):
        if DBG_NOROPE:
            return store_fm(c, src_ap, src_reads, M, n, row0, t0)
        Pm = rp128 if which == 128 else rp64
        rt = rope_tabs(c, which, t0, n)
        if ROPE_STEPS == 0:
            return store_fm(c, src_ap, list(src_reads) + [rt.r], M, n, row0, t0)
        qa = c.qaR.next()
        p.op("act", lambda e: e.activation(out=qa.t[0:M, 0:n], in_=src_ap, func=AF.Copy), reads=src_reads, writes=[qa.r])
        if ROPE_STEPS == 1:
            return store_fm(c, src_ap, list(src_reads) + [rt.r, qa.r], M, n, row0, t0)
        t1 = c.t1R.next()
        p.op("dve", lambda e: e.tensor_tensor(out=t1.t[0:M, 0:n], in0=qa.t[0:M, 0:n], in1=rt.t[0:M, 0, 0:n], op=ALU.mult),
             reads=[qa.r, rt.r], writes=[t1.r])
        if ROPE_STEPS == 2:
            return store_fm(c, t1.t[0:M, 0:n], [t1.r, qa.r], M, n, row0, t0)
        ps2, pr2 = c.psP.next()
        p.op("pe", lambda e: e.matmul(ps2[0:M, 0:n], lhsT=Pm[0:M, 0:M], rhs=qa.t[0:M, 0:n], start=True, stop=True),
             reads=[qa.r, cst_r], writes=[pr2])
        if ROPE_STEPS == 3:
            return store_fm(c, ps2[0:M, 0:n], [t1.r, pr2], M, n, row0, t0)
        t2 = c.t2R.next()
        p.op("dve", lambda e: e.scalar_tensor_tensor(out=t2.t[0:M, 0:n], in0=ps2[0:M, 0:n], scalar=1.0, in1=rt.t[0:M, 1, 0:n], op0=ALU.mult, op1=ALU.mult),
             reads=[pr2, rt.r], writes=[t2.r])
        o = c.osR.next()
        p.op(ROPE_ADD_ENG, lambda e: e.tensor_tensor(out=o.t[0:M, 0:n], in0=t1.t[0:M, 0:n], in1=t2.t[0:M, 0:n], op=ALU.add),
             reads=[t1.r, t2.r], writes=[o.r])
        p.dma("sp", dma1(FM_d[row0:row0 + M, t0:t0 + n], o.t[0:M, 0:n]), o.ds, 1, reads=[o.r], awrites=[FM_r])

    def fm_rmsnorm(c, pss, nfeat, n, gain_cols, dsts, dst_res_list):
        pz, pzr = c.psP.next()
        npss = len(pss)
        for j, (ps, pr) in enumerate(pss):
            sq = c.sqR.next()
            p.op("act", lambda e, ps=ps, sq=sq: e.activation(out=sq.t[:, 0:n], in_=ps[:, 0:n], func=AF.Square), reads=[pr], writes=[sq.r])
            p.op("pe", lambda e, sq=sq, j=j: e.matmul(pz[:, 0:n], lhsT=ones_b[:], rhs=sq.t[:, 0:n], start=(j == 0), stop=(j == npss - 1)),
                 reads=[sq.r, cst_r], writes=[pzr])
        tm = c.tmR.next()
        p.op("act", lambda e: e.activation(out=tm.t[:, 0:n], in_=pz[:, 0:n], func=AF.Sqrt, scale=1.0 / nfeat, bias=EPS), reads=[pzr], writes=[tm.r])
        rb = c.rbR.next()
        p.op("dve", lambda e: e.reciprocal(out=rb.t[:, 0:n], in_=tm.t[:, 0:n]), reads=[tm.r], writes=[rb.r])
        for j, (ps, pr) in enumerate(pss):
            p.op("dve", lambda e, ps=ps, j=j: e.scalar_tensor_tensor(out=dsts[j], in0=ps[:, 0:n], scalar=gain_cols[j], in1=rb.t[:, 0:n],
                                                                    op0=ALU.mult, op1=ALU.mult),
                 reads=[pr, rb.r, small_r], awrites=[dst_res_list[j]])

    def tm_proj(c, lhs_src, lhs_reads, nk, rhs_fn, rhs_reads, ncols, col0, tiles):
        for i in tiles:
            ps, pr = c.psV.next()

            def f(e, ps=ps, i=i):
                for k in range(nk):
                    r = e.matmul(ps[:, 0:ncols], lhsT=lhs_src[:, k, i * 128:(i + 1) * 128], rhs=rhs_fn(k), start=(k == 0), stop=(k == nk - 1))
                return r
            p.op("pe", f, reads=list(lhs_reads) + list(rhs_reads), writes=[pr])
            o = c.osR.next()
            eng = "act" if c.ev % 2 == 0 else "dve"
            c.ev += 1
            if eng == "act":
                p.op("act", lambda e, ps=ps, o=o: e.activation(out=o.t[:, 0:ncols], in_=ps[:, 0:ncols], func=AF.Copy), reads=[pr], writes=[o.r])
            else:
                p.op("dve", lambda e, ps=ps, o=o: e.tensor_copy(out=o.t[:, 0:ncols], in_=ps[:, 0:ncols]), reads=[pr], writes=[o.r])
            p.dma("sp", dma1(TM_d[i * 128:(i + 1) * 128, col0:col0 + ncols], o.t[:, 0:ncols]), o.ds, 1, reads=[o.r], awrites=[TM_r])

    def col_load(dst_ap, src_1d, eng="sp"):
        p.dma(eng, dma1(dst_ap, src_1d.rearrange("(c p) -> p c", p=128), **NCG), "sm", 1, writes=[small_r])

    def phase2_ab(es, l, li, big, big_rs, tiles_tok):
        c = proj_setup(es)
        w_in = I("ab_w_in")[li]
        cqnT = AS(es, "cqnT", [128, 4, T], BF16)
        ckvnT = AS(es, "ckvnT", [128, 2, T], BF16)
        cq_rs = [Res(f"cqn{j}") for j in range(4)]
        ckv_rs = [Res(f"ckvn{j}") for j in range(2)]
        gq = AS(es, "gq", [128, 8], F32)
        col_load(gq[:, 0:4], I("ab_q_norm")[li])
        col_load(gq[:, 4:6], I("ab_kv_norm")[li])
        tblks = [tb for tb in TBLK if tb[0] < S or len(tiles_tok) > 16]
        chunks = [(0, 512), (512, 512), (1024, 512), (1536, 512), (2048, 320)]
        for ci, (c0, cn) in enumerate(chunks[:P2_LIMIT]):
            wb, wv = wload(w_in[:, c0:c0 + cn], cn, 16)
            rd = big_rs + [wb.r]
            for (t0, tn) in tblks:
                rhs = lambda k, t0=t0, tn=tn: big[:, k, t0:t0 + tn]
                if ci in (0, 1):
                    for m in range(4):
                        h = ci * 4 + m
                        ps, pr = fm_matmul(c, lambda k, m=m, wv=wv: wv[:, k, m * 128:(m + 1) * 128], 16, rhs, 128, tn, rd)
                        rope_store(c, ps[:, 0:tn], [pr], 128, tn, t0, 128, h * 128)
                elif ci == 2:
                    for m in range(2):
                        ps, pr = fm_matmul(c, lambda k, m=m, wv=wv: wv[:, k, m * 128:(m + 1) * 128], 16, rhs, 128, tn, rd)
                        rope_store(c, ps[:, 0:tn], [pr], 128, tn, t0, 128, 1024 + m * 128)
                elif ci == 3:
                    pss = [fm_matmul(c, lambda k, m=m, wv=wv: wv[:, k, m * 128:(m + 1) * 128], 16, rhs, 128, tn, rd) for m in range(4)]
                    fm_rmsnorm(c, pss, 512, tn, [gq[:, j:j + 1] for j in range(4)], [cqnT[:, j, t0:t0 + tn] for j in range(4)], cq_rs)
                else:
                    pss = [fm_matmul(c, lambda k, m=m, wv=wv: wv[:, k, m * 128:(m + 1) * 128], 16, rhs, 128, tn, rd) for m in range(2)]
                    fm_rmsnorm(c, pss, 256, tn, [gq[:, 4 + j:5 + j] for j in range(2)], [ckvnT[:, j, t0:t0 + tn] for j in range(2)], ckv_rs)
                    ps, pr = fm_matmul(c, lambda k, wv=wv: wv[:, k, 256:320], 16, rhs, 64, tn, rd)
                    rope_store(c, ps[0:64, 0:tn], [pr], 64, tn, t0, 64, 3840)
            if ci == 2:
                tm_proj(c, big, big_rs, 16, lambda k, wv=wv: wv[:, k, 256:512], [wb.r], 256, 0, tiles_tok)
        if P2_LIMIT < 6:
            return
        wb, wv = wload(I("ab_w_uq")[li], 1536, 4)
        for h in range(8):
            for (t0, tn) in tblks:
                rhs = lambda k, t0=t0, tn=tn: cqnT[:, k, t0:t0 + tn]
                ps, pr = fm_matmul(c, lambda k, h=h, wv=wv: wv[:, k, h * 192:h * 192 + 128], 4, rhs, 128, tn, cq_rs + [wb.r])
                store_fm(c, ps[:, 0:tn], [pr], 128, tn, 1280 + h * 128, t0)
                ps, pr = fm_matmul(c, lambda k, h=h, wv=wv: wv[:, k, h * 192 + 128:h * 192 + 192], 4, rhs, 64, tn, cq_rs + [wb.r])
                rope_store(c, ps[0:64, 0:tn], [pr], 64, tn, t0, 64, 2304 + h * 64)
        wb, wv = wload(I("ab_w_ukv")[li], 2048, 2)
        wv4 = wb.t[:, 0:4096].rearrange("p (c h x) -> p c h x", c=2, h=8)
        for h in range(8):
            for (t0, tn) in tblks:
                rhs = lambda k, t0=t0, tn=tn: ckvnT[:, k, t0:t0 + tn]
                ps, pr = fm_matmul(c, lambda k, h=h, wv=wv: wv[:, k, h * 256:h * 256 + 128], 2, rhs, 128, tn, ckv_rs + [wb.r])
                store_fm(c, ps[:, 0:tn], [pr], 128, tn, 2816 + h * 128, t0)
        for half in range(2):
            tm_proj(c, ckvnT, ckv_rs, 2, lambda k, half=half, wv4=wv4: wv4[:, k, half * 4:half * 4 + 4, 128:256], [wb.r], 512, 256 + half * 512, tiles_tok)

    class AttCtx:
        pass

    def att_setup(es, mla=True):
        a = AttCtx()
        a.psS = PsRing([0, 1, 2])
        a.psO = PsRing([3, 4])
        a.psZ = PsRing([5, 6])
        a.ptR = ringS(es, "apt", [128, 512], BF16, 4)
        a.rsR = ringS(es, "ars", [128, 512], F32, 1)
        a.r2R = ringS(es, "ar2", [128, 512], F32, 1)
        a.obR = ringS(es, "aob", [128, 512], F32, 1)
        a.qR = ringS(es, "aq", [128, T], BF16, 2)
        if mla:
            a.q2R = ringS(es, "aq2", [64, T], BF16, 2)
        a.kR = ringS(es, "ak", [128, T], BF16, 2)
        if mla:
            a.k2R = ringS(es, "ak2", [64, T], BF16, 1)
        a.vR = ringS(es, "av", [128, NT, 128], BF16, 2)
        a.band = AS(es, "band", [128, 384], BF16)
        a.nbrm = AS(es, "nbrm", [128, NPAT, 128], BF16)
        a.c_r = Res("attc")
        p.dma("sp", dma1(a.band[:], I("c_band")), "sm2", 1, writes=[a.c_r])
        p.dma("sp", dma1(a.nbrm[:], I("c_nbrm")), "sm2", 1, awrites=[a.c_r])
        return a

    def att_load_fm(ring, row0, M):
        s = ring.next()
        p.dma("sp", dma1(s.t[0:M, :], FM_d[row0:row0 + M, :]), s.ds, 1, reads=[FM_r], writes=[s.r])
        return s

    def att_load_v(a, col0):
        s = a.vR.next()
        p.dma("sp", dma1(s.t[:], TM_d[:, col0:col0 + 128].rearrange("(i p) v -> p i v", p=128)), s.ds, 1, reads=[TM_r], writes=[s.r])
        return s

    def attend(a, qparts, kparts, qk_reads, vs, q0, qn, tiles, scale, dst, dst_res, sink_ap=None, sink_reads=()):
        ATT_COUNT[0] += 1
        if ATT_COUNT[0] > ATT_LIMIT:
            return
        po, por = a.psO.next()
        pz, pzr = a.psZ.next()
        nt = len(tiles)
        sT = [None] * nt
        npart = len(qparts)

        def qk(i):
            kt, qa, qb, E, Er = tiles[i]
            ps, pr = a.psS.next()

            def f(e):
                for j in range(npart):
                    M = qparts[j][1]
                    r = e.matmul(ps[:, 0:qb - qa], lhsT=kparts[j][0][0:M, kt * 128:(kt + 1) * 128],
                                 rhs=qparts[j][0][0:M, q0 + qa:q0 + qb], start=(j == 0), stop=(j == npart - 1))
                return r
            p.op("pe", f, reads=qk_reads, writes=[pr])
            sT[i] = (ps, pr)

        PIPE = 2
        for i in range(min(PIPE, nt)):
            qk(i)
        for i in range(nt):
            kt, qa, qb, E, Er = tiles[i]
            n = qb - qa
            ps, pr = sT[i]
            pt = a.ptR.next()
            if ATT_STEPS < 2:
                if i + PIPE < nt:
                    qk(i + PIPE)
                continue
            p.op("act", lambda e, ps=ps, pt=pt, n=n: e.activation(out=pt.t[:, 0:n], in_=ps[:, 0:n], func=AF.Exp, scale=scale),
                 reads=[pr], writes=[pt.r])
            if E is not None and ATT_STEPS >= 3:
                p.op("dve", lambda e, pt=pt, n=n, E=E: e.tensor_tensor(out=pt.t[:, 0:n], in0=pt.t[:, 0:n], in1=E, op=ALU.mult),
                     reads=[pt.r] + list(Er), writes=[pt.r])
            if i + PIPE < nt:
                qk(i + PIPE)
            if ATT_STEPS < 4:
                continue

            def pv(e, pt=pt, kt=kt, qa=qa, qb=qb, n=n, i=i):
                e.matmul(po[:, qa:qb], lhsT=vs.t[:, kt, :], rhs=pt.t[:, 0:n], start=(i == 0), stop=(i == nt - 1))
                return e.matmul(pz[:, qa:qb], lhsT=ones_b[:], rhs=pt.t[:, 0:n], start=(i == 0), stop=(i == nt - 1))
            p.op("pe", pv, reads=[pt.r, vs.r, cst_r], writes=[por, pzr])
        if ATT_STEPS < 5:
            return
        rs = a.rsR.next()
        if sink_ap is not None:
            p.op("act", lambda e: e.activation(out=rs.t[:, 0:qn], in_=pz[:, 0:qn], func=AF.Identity, bias=sink_ap, scale=1.0),
                 reads=[pzr] + list(sink_reads), writes=[rs.r])
        else:
            p.op("act", lambda e: e.activation(out=rs.t[:, 0:qn], in_=pz[:, 0:qn], func=AF.Copy), reads=[pzr], writes=[rs.r])
        r2 = a.r2R.next()
        p.op("dve", lambda e: e.reciprocal(out=r2.t[:, 0:qn], in_=rs.t[:, 0:qn]), reads=[rs.r], writes=[r2.r])
        ob = a.obR.next()
        p.op("act", lambda e: e.activation(out=ob.t[:, 0:qn], in_=po[:, 0:qn], func=AF.Copy), reads=[por], writes=[ob.r])
        p.op("dve", lambda e: e.tensor_tensor(out=dst, in0=ob.t[:, 0:qn], in1=r2.t[:, 0:qn], op=ALU.mult),
             reads=[ob.r, r2.r], awrites=[dst_res])

    def qblocks(need_ctx):
        return [tb for tb in TBLK if tb[0] < S or need_ctx]

    CTX_TILES = [(16, None, None), (17, None, None)]

    def phase3_ab(es, l, li, big, big_rs, need_ctx):
        a = att_setup(es)
        snk = AS(es, "snk", [128, 8], F32)
        snk_r = Res("snk")
        p.dma("sp", dma1(snk[:], bcast_rows(I("ab_sink")[li:li + 1, :], 128)), "sm2", 1, writes=[snk_r])
        p.op("act", lambda e: e.activation(out=snk[:], in_=snk[:], func=AF.Exp), reads=[snk_r], writes=[snk_r])
        sc_a = 128 ** -0.5
        sc_b = 192 ** -0.5
        for kv in range(2):
            ks = att_load_fm(a.kR, 1024 + kv * 128, 128)
            vs = att_load_v(a, kv * 128)
            for g in range(4):
                h = kv * 4 + g
                qs = att_load_fm(a.qR, h * 128, 128)
                for (q0, qn) in qblocks(need_ctx):
                    tiles = [(16, 0, qn, None, ()), (17, 0, qn, None, ())]
                    if q0 < S:
                        for kt in range(max(0, q0 // 128 - 1), min(15, (q0 + qn) // 128) + 1):
                            lo = max(q0, kt * 128 - 128)
                            hi = min(q0 + qn, kt * 128 + 256)
                            if hi <= lo:
                                continue
                            m0 = lo - (kt * 128 - 128)
                            tiles.append((kt, lo - q0, hi - q0, a.band[:, m0:m0 + hi - lo], (a.c_r,)))
                    attend(a, [(qs.t, 128)], [(ks.t, 128)], [qs.r, ks.r], vs, q0, qn, tiles, sc_a,
                           big[:, h, q0:q0 + qn], big_rs[h], sink_ap=snk[:, h:h + 1], sink_reads=(snk_r,))
        kr = att_load_fm(a.k2R, 3840, 64)
        for h in range(8):
            ks = att_load_fm(a.kR, 2816 + h * 128, 128)
            vs = att_load_v(a, 256 + h * 128)
            qs = att_load_fm(a.qR, 1280 + h * 128, 128)
            q2 = att_load_fm(a.q2R, 2304 + h * 64, 64)
            for (q0, qn) in qblocks(need_ctx):
                kts = list(range(16, 18)) + (list(range(16)) if q0 < S else [])
                tiles = [(kt, 0, qn, None, ()) for kt in kts]
                attend(a, [(qs.t, 128), (q2.t, 64)], [(ks.t, 128), (kr.t, 64)], [qs.r, q2.r, ks.r, kr.r], vs, q0, qn, tiles, sc_b,
                       big[:, 8 + h, q0:q0 + qn], big_rs[8 + h])

    def phase2_cd(es, l, li, big, big_rs, tiles_tok):
        c = proj_setup(es)
        c.qnR = ringS(es, "pqn", [128, 512], F32, 2)
        w_in = I("cd_w_in")[li]
        gq = AS(es, "gqc", [128, 2], F32)
        col_load(gq[:, 0:1], I("cd_q_norm")[li])
        col_load(gq[:, 1:2], I("cd_k_norm")[li])
        tblks = [tb for tb in TBLK if tb[0] < S or len(tiles_tok) > 16]
        for ci in range(9):
            wb, wv = wload(w_in[:, ci * 512:(ci + 1) * 512], 512, 16)
            rd = big_rs + [wb.r]
            if ci in (7, 8):
                tm_proj(c, big, big_rs, 16, lambda k, wv=wv: wv[:, k, :], [wb.r], 512, 256 + (ci - 7) * 512, tiles_tok)
                continue
            for (t0, tn) in tblks:
                rhs = lambda k, t0=t0, tn=tn: big[:, k, t0:t0 + tn]
                nm = 2 if ci == 2 else 4
                for m in range(nm):
                    ps, pr = fm_matmul(c, lambda k, m=m, wv=wv: wv[:, k, m * 128:(m + 1) * 128], 16, rhs, 128, tn, rd)
                    if ci in (0, 1, 2):
                        row0 = (ci * 4 + m) * 128 if ci < 2 else 1024 + m * 128
                        g = gq[:, 0:1] if ci < 2 else gq[:, 1:2]
                        qn = c.qnR.next()
                        fm_rmsnorm(c, [(ps, pr)], 128, tn, [g], [qn.t[:, 0:tn]], [qn.r])
                        rope_store(c, qn.t[:, 0:tn], [qn.r], 128, tn, t0, 128, row0)
                    elif ci in (3, 4):
                        store_fm(c, ps[:, 0:tn], [pr], 128, tn, 1280 + ((ci - 3) * 4 + m) * 128, t0)
                    else:
                        store_fm(c, ps[:, 0:tn], [pr], 128, tn, 2304 + ((ci - 5) * 4 + m) * 128, t0)
            if ci == 2:
                tm_proj(c, big, big_rs, 16, lambda k, wv=wv: wv[:, k, 256:512], [wb.r], 256, 0, tiles_tok)

    def phase3_cd(es, l, li, big, big_rs, need_ctx):
        a = att_setup(es, mla=False)
        sc = 128 ** -0.5
        for kv in range(2):
            ks = att_load_fm(a.kR, 1024 + kv * 128, 128)
            vs = att_load_v(a, kv * 128)
            for g in range(4):
                h = kv * 4 + g
                qs = att_load_fm(a.qR, h * 128, 128)
                for (q0, qn) in qblocks(need_ctx):
                    kts = list(range(16, 18)) + (list(range(16)) if q0 < S else [])
                    tiles = [(kt, 0, qn, None, ()) for kt in kts]
                    attend(a, [(qs.t, 128)], [(ks.t, 128)], [qs.r, ks.r], vs, q0, qn, tiles, sc, big[:, h, q0:q0 + qn], big_rs[h])
        Jm = AS(es, "Jm", [128, 128], F32)
        padt = AS(es, "padt", [120, 160], F32)
        pad_r = Res("padt")
        p.dma("sp", dma1(Jm[:], I("c_J")), "sm2", 1, writes=[a.c_r])
        p.op("dve", lambda e: e.memset(padt[:], 0.0), writes=[pad_r])
        p.dma("sp", dma1(padt[:, 64:95], I("cd_rpb")[li].rearrange("h r c -> (h r) c")), "sm2", 1, writes=[pad_r])
        p.dma("sp", dma1(rpbpad_d[:, :], padt[:]), "sm2", 1, reads=[pad_r], writes=[rpb_r])
        keys = sorted({(d_, pt_) for ent in NBR_PLAN for (_, d_, pt_) in ent})
        deltas = sorted({d_ for d_, _ in keys})
        kidx = {k_: i for i, k_ in enumerate(keys)}
        egR = ringS(es, "eg", [128, len(keys), 128], BF16, 2)
        tTR = ringS(es, "egT", [128, 128], F32, 2)
        exR = ringS(es, "egx", [128, 128], F32, 2)
        pe_ps, pe_pr = psb[7], psr[7]
        for h in range(8):
            eg = egR.next()
            first = True
            for d_ in deltas:
                tT = tTR.next()
                nd = 0
                for qr in range(2):
                    for kr in range(2):
                        dr = min(14, max(0, 2 * d_ + kr - qr + 7))
                        src = bass.AP(rpbpad_d.tensor, (h * 15 + dr) * 160 + 16, [[1, 64], [1, 64]])
                        kw = dict(writes=[tT.r]) if nd == 0 else dict(awrites=[tT.r])
                        p.dma("sp", dma1(tT.t[qr * 64:(qr + 1) * 64, kr * 64:(kr + 1) * 64], src), tT.ds, 1, reads=[rpb_r], **kw)
                        nd += 1
                p.op("pe", lambda e, tT=tT: e.matmul(pe_ps[:, 0:128], lhsT=tT.t[:], rhs=Jm[:], start=True, stop=True),
                     reads=[tT.r, a.c_r], writes=[pe_pr])
                ex = exR.next()
                p.op("act", lambda e, ex=ex: e.activation(out=ex.t[:], in_=pe_ps[:, 0:128], func=AF.Exp), reads=[pe_pr], writes=[ex.r])
                for (dd, pt_) in keys:
                    if dd != d_:
                        continue
                    ki = kidx[(dd, pt_)]
                    kw = dict(writes=[eg.r]) if first else dict(awrites=[eg.r])
                    first = False
                    p.op("dve", lambda e, ex=ex, eg=eg, ki=ki, pt_=pt_: e.tensor_tensor(out=eg.t[:, ki, :], in0=ex.t[:], in1=a.nbrm[:, pt_, :], op=ALU.mult),
                         reads=[ex.r, a.c_r], **kw)
            ks = att_load_fm(a.kR, 2304 + h * 128, 128)
            vs = att_load_v(a, 256 + h * 128)
            qs = att_load_fm(a.qR, 1280 + h * 128, 128)
            for qt in range(16):
                tiles = [(16, 0, 128, None, ()), (17, 0, 128, None, ())]
                for (kt, d_, pt_) in NBR_PLAN[qt]:
                    tiles.append((kt, 0, 128, eg.t[:, kidx[(d_, pt_)], :], (eg.r,)))
                attend(a, [(qs.t, 128)], [(ks.t, 128)], [qs.r, ks.r], vs, qt * 128, 128, tiles, sc, big[:, 8 + h, qt * 128:(qt + 1) * 128], big_rs[8 + h])
            if need_ctx:
                tiles = [(16, 0, LC, None, ()), (17, 0, LC, None, ())]
                attend(a, [(qs.t, 128)], [(ks.t, 128)], [qs.r, ks.r], vs, S, LC, tiles, sc, big[:, 8 + h, S:T], big_rs[8 + h])

    def phase4(es, l, w_out, big, big_rs, need_ctx):
        ntile = NT if need_ctx else 16
        nr = 2 if need_ctx else 1
        wch = [wload(w_out[:, n * 512:(n + 1) * 512], 512, 16) for n in range(4)]
        g1b = AS(es, "g1b", [128, D], F32)
        g1_r = Res("g1b")
        Am, Bm, am_r = make_AB(es, l, 2)
        wr_sb = AS(es, "wr_sb", [128, 16, NE], F32)
        wrA = AS(es, "wrA", [128, 2, 16, NE], F32)
        biasr = AS(es, "biasr", [1, 2, NE], F32)
        biasb = AS(es, "biasb", [128, 2, NE], F32)
        wr_r = Res("wr")
        p.dma("sp", dma1(wr_sb[:], I("w_router")[l].rearrange("(c k) e -> k c e", k=128)), "sm2", 1, writes=[wr_r])
        for r in range(nr):
            for c in range(16):
                p.op("dve", lambda e, r=r, c=c: e.tensor_scalar(out=wrA[:, r, c, :], in0=wr_sb[:, c, :], scalar1=Am[:, c, r:r + 1], scalar2=None, op0=ALU.mult),
                     reads=[wr_r, am_r], awrites=[wr_r])
            pb, pbr = psb[7], psr[7]

            def fb(e, r=r):
                for c in range(16):
                    q = e.matmul(pb[0:1, 0:NE], lhsT=Bm[:, c, r:r + 1], rhs=wr_sb[:, c, :], start=(c == 0), stop=(c == 15))
                return q
            p.op("pe", fb, reads=[wr_r, modcol_r[l]], writes=[pbr])
            p.op("dve", lambda e, r=r: e.tensor_copy(out=biasr[0:1, r, :], in_=pb[0:1, 0:NE]), reads=[pbr], awrites=[wr_r])
            p.op("pe", lambda e, r=r: e.matmul(pb[:, 0:NE], lhsT=ones_f[0:1, :], rhs=biasr[0:1, r, :], start=True, stop=True),
                 reads=[wr_r, cst_r], writes=[pbr])
            p.op("dve", lambda e, r=r: e.tensor_copy(out=biasb[:, r, :], in_=pb[:, 0:NE]), reads=[pbr], awrites=[wr_r])
        xtR = ringS(es, "p4x", [128, D], F32, 2)
        tR = ringS(es, "p4t", [128, 512], F32, 2)
        xnbR = ringS(es, "p4nb", [128, D], BF16, 2)
        xTR = ringS(es, "p4xT", [128, 16, 128], F32, 1)
        ssR = ringS(es, "p4s", [128, 4], F32, 3)
        smR = ringS(es, "p4sm", [128, 40], F32, 2)
        psA = PsRing([0, 1, 2, 3])
        psT = PsRing([4, 5])
        for i in range(ntile):
            r = 0 if i < 16 else 1
            if i == 0 or i == 16:
                p.dma("sp", dma1(g1b[:], bcast_rows(modrow_d[l, r:r + 1, 2 * D:3 * D], 128)), "sm2", 1, reads=[modrow_r], writes=[g1_r])
            xt = xtR.next()
            p.dma("sp", dma1(xt.t[:], xsrc(l, i)), xt.ds, 1, reads=[xres_r[i]], writes=[xt.r])
            for n in range(4):
                wb, wv = wch[n]
                ps, pr = psA.next()

                def f(e, ps=ps, wv=wv, i=i):
                    for k in range(16):
                        q = e.matmul(ps[:, :], lhsT=big[:, k, i * 128:(i + 1) * 128], rhs=wv[:, k, :], start=(k == 0), stop=(k == 15))
                    return q
                p.op("pe", f, reads=big_rs + [wb.r], writes=[pr])
                t = tR.next()
                p.op("dve", lambda e, ps=ps, t=t, n=n: e.scalar_tensor_tensor(out=t.t[:], in0=ps[:, :], scalar=1.0, in1=g1b[:, n * 512:(n + 1) * 512], op0=ALU.mult, op1=ALU.mult),
                     reads=[pr, g1_r], writes=[t.r])
                p.op("pool", lambda e, xt=xt, t=t, n=n: e.tensor_tensor(out=xt.t[:, n * 512:(n + 1) * 512], in0=xt.t[:, n * 512:(n + 1) * 512], in1=t.t[:], op=ALU.add),
                     reads=[t.r, xt.r], writes=[xt.r])
            p.dma("sp", dma1(xres[i * 128:(i + 1) * 128, :], xt.t[:]), xt.ds, 1, reads=[xt.r], writes=[xres_r[i]])
            xnb = xnbR.next()
            ss = norm_tile(xt, ssR, xnb.t, xnb.r)
            p.op("act", lambda e, xt=xt, ss=ss, xnb=xnb: e.activation(out=xnb.t[:], in_=xt.t[:], func=AF.Copy, scale=ss.t[:, 2:3]),
                 reads=[xt.r, ss.r], writes=[xnb.r])
            p.dma("sp", dma1(xn_d[i * 128:(i + 1) * 128, :], xnb.t[:]), xnb.ds, 1, reads=[xnb.r], awrites=[xn_r])
            xT = xTR.next()
            for g4 in range(4):
                pt, ptr = psT.next()

                def tr(e, pt=pt, xt=xt, g4=g4):
                    for j in range(4):
                        cc = g4 * 4 + j
                        q = e.transpose(out=pt[:, j * 128:(j + 1) * 128], in_=xt.t[:, cc * 128:(cc + 1) * 128], identity=ident_f[:])
                    return q
                p.op("pe", tr, reads=[xt.r, cst_r], writes=[ptr])
                dstT = xT.t[:, g4 * 4:(g4 + 1) * 4, :].rearrange("p c t -> p (c t)")
                if g4 % 2 == 0:
                    p.op("act", lambda e, pt=pt, dstT=dstT: e.activation(out=dstT, in_=pt[:, :], func=AF.Copy), reads=[ptr], awrites=[xT.r])
                else:
                    p.op("dve", lambda e, pt=pt, dstT=dstT: e.tensor_copy(out=dstT, in_=pt[:, :]), reads=[ptr], awrites=[xT.r])
            pl, plr = psb[6], psr[6]

            def fr(e, xT=xT, r=r):
                for c in range(16):
                    q = e.matmul(pl[:, 0:NE], lhsT=xT.t[:, c, :], rhs=wrA[:, r, c, :], start=(c == 0), stop=(c == 15))
                return q
            p.op("pe", fr, reads=[xT.r, wr_r], writes=[plr])
            sm = smR.next()
            p.op("dve", lambda e, sm=sm, ss=ss, r=r: e.scalar_tensor_tensor(out=sm.t[:, 8:8 + NE], in0=pl[:, 0:NE], scalar=ss.t[:, 2:3], in1=biasb[:, r, :],
                                                                         op0=ALU.mult, op1=ALU.add),
                 reads=[plr, ss.r, wr_r], writes=[sm.r])
            p.op("dve", lambda e, sm=sm: e.tensor_reduce(out=sm.t[:, 0:1], in_=sm.t[:, 8:8 + NE], axis=mybir.AxisListType.X, op=ALU.max),
                 reads=[sm.r], writes=[sm.r])
            p.op("dve", lambda e, sm=sm: e.tensor_scalar(out=sm.t[:, 1:2], in0=sm.t[:, 0:1], scalar1=-1.0, scalar2=None, op0=ALU.mult),
                 reads=[sm.r], writes=[sm.r])
            p.op("act", lambda e, sm=sm: e.activation(out=sm.t[:, 8:8 + NE], in_=sm.t[:, 8:8 + NE], func=AF.Exp, bias=sm.t[:, 1:2], accum_out=sm.t[:, 2:3]),
                 reads=[sm.r], writes=[sm.r])
            p.op("dve", lambda e, sm=sm: e.reciprocal(out=sm.t[:, 3:4], in_=sm.t[:, 2:3]), reads=[sm.r], writes=[sm.r])
            p.op("dve", lambda e, sm=sm: e.tensor_scalar(out=sm.t[:, 24:24 + NE], in0=sm.t[:, 8:8 + NE], scalar1=sm.t[:, 3:4], scalar2=None, op0=ALU.mult),
                 reads=[sm.r], writes=[sm.r])
            pa, par = psb[7], psr[7]
            p.op("pe", lambda e, sm=sm: e.transpose(out=pa[0:NE, 0:128], in_=sm.t[:, 24:24 + NE], identity=ident_f[:]), reads=[sm.r, cst_r], writes=[par])
            p.op("dve", lambda e, i=i: e.tensor_copy(out=affT[:, i * 128:(i + 1) * 128], in_=pa[0:NE, 0:128]), reads=[par], awrites=[affT_r])

    def phase5(es, l, need_ctx):
        ncap = CAPT if need_ctx else CAP_L
        chunks = [(0, 128), (128, 128)] + ([(256, 32)] if need_ctx else [])
        Am, Bm, am_r = make_AB(es, l, 2)
        work = AS(es, "tkw", [NE, T], F32)
        mx = AS(es, "tkmx", [NE, CAPT], F32)
        ix = AS(es, "tkix", [NE, CAPT], U32)
        ixf = AS(es, "tkixf", [NE, CAPT], F32)
        tk_r = Res("topk")
        p.op("dve", lambda e: e.tensor_copy(out=work[:], in_=affT[:]), reads=[affT_r], writes=[tk_r])
        segs = [(0, S, 0, CAP_L // 8)] + ([(S, LC, CAP_L, CAP_C // 8)] if need_ctx else [])
        for (c0, cn, o0, rounds) in segs:
            for rd in range(rounds):
                sl = slice(o0 + rd * 8, o0 + rd * 8 + 8)
                p.op("dve", lambda e, sl=sl, c0=c0, cn=cn: e.max(out=mx[:, sl], in_=work[:, c0:c0 + cn]), reads=[tk_r], writes=[tk_r])
                p.op("dve", lambda e, sl=sl, c0=c0, cn=cn: e.max_index(out=ix[:, sl], in_max=mx[:, sl], in_values=work[:, c0:c0 + cn]), reads=[tk_r], writes=[tk_r])
                p.op("dve", lambda e, sl=sl, c0=c0, cn=cn: e.match_replace(out=work[:, c0:c0 + cn], in_to_replace=mx[:, sl], in_values=work[:, c0:c0 + cn], imm_value=-1.0),
                     reads=[tk_r], writes=[tk_r])
        p.op("dve", lambda e: e.tensor_copy(out=ixf[:, 0:ncap], in_=ix[:, 0:ncap]), reads=[tk_r], writes=[tk_r])
        if need_ctx:
            p.op("dve", lambda e: e.tensor_scalar(out=ixf[:, CAP_L:CAPT], in0=ixf[:, CAP_L:CAPT], scalar1=float(S), scalar2=None, op0=ALU.add),
                 reads=[tk_r], writes=[tk_r])
        idxT = AS(es, "idxT", [128, 3, NE], I32)
        gT = AS(es, "gT", [128, 3, NE], F32)
        for j, (a0, an) in enumerate(chunks):
            for src, dstt in ((ixf, idxT), (mx, gT)):
                pa, par = psb[7], psr[7]
                p.op("pe", lambda e, src=src, a0=a0, an=an: e.transpose(out=pa[0:an, 0:NE], in_=src[0:NE, a0:a0 + an], identity=ident_f[0:NE, 0:NE]),
                     reads=[tk_r, cst_r], writes=[par])
                p.op("dve", lambda e, dstt=dstt, j=j, an=an: e.tensor_copy(out=dstt[0:an, j, :], in_=pa[0:an, 0:NE]), reads=[par], awrites=[tk_r])
        g2b = AS(es, "g2b", [128, 2, D], F32)
        g2_r = Res("g2b")
        for r in range(2 if need_ctx else 1):
            p.dma("sp", dma1(g2b[:, r, :], bcast_rows(modrow_d[l, r:r + 1, 5 * D:6 * D], 128)), "sm2", 1, reads=[modrow_r], writes=[g2_r])
        if MOE_STEPS < 2:
            return
        xinR = ringS(es, "mxin", [128, 3, D], BF16, 2)
        xTR = ringS(es, "mxT", [128, 16, CAPT], BF16, 2)
        hidR = ringS(es, "mhid", [128, 8, CAPT], BF16, 2)
        sgR = ringS(es, "msg", [128, CAPT], F32, 2)
        uuR = ringS(es, "muu", [128, CAPT], F32, 2)
        toR = ringS(es, "mto", [128, 512], F32, 2)
        ostR = ringS(es, "most", [128, 3, D], F32, 1)
        psT = PsRing([0, 1])
        psG = PsRing([2, 3])
        psU = PsRing([4, 5])
        psD = PsRing([6, 7])
        wg_all = I("w_gate")[l]
        wu_all = I("w_up")[l]
        wd_all = I("w_down")[l]
        all_x = list(xres_r)

        def gather(e_):
            xin = xinR.next()
            for j, (a0, an) in enumerate(chunks):
                p.dma("pool", lambda e, xin=xin, j=j, an=an, e_=e_: [e.indirect_dma_start(
                    out=xin.t[0:an, j, :], out_offset=None, in_=xn_d[:, :],
                    in_offset=bass.IndirectOffsetOnAxis(ap=idxT[0:an, j, e_:e_ + 1], axis=0))],
                    xin.ds, 1, reads=[xn_r, tk_r], awrites=[xin.r])
            return xin

        def wunit_gu(e_, jb):
            wb = Wr.next()
            wvg = wb.t[:, 0:4096].rearrange("p (c n) -> p c n", c=16)
            wvu = wb.t[:, 4096:8192].rearrange("p (c n) -> p c n", c=16)
            p.dma("pool", dma1(wvg, wg_all[e_, :, jb * 256:(jb + 1) * 256].rearrange("(c k) n -> k c n", k=128)), wb.ds, 1, writes=[wb.r])
            p.dma("pool", dma1(wvu, wu_all[e_, :, jb * 256:(jb + 1) * 256].rearrange("(c k) n -> k c n", k=128)), wb.ds, 1, awrites=[wb.r])
            return wb, wvg, wvu

        def wunit_d(e_, db):
            wb = Wr.next()
            wvd = wb.t[:, 0:4096].rearrange("p (c n) -> p c n", c=8)
            p.dma("pool", dma1(wvd, wd_all[e_, :, db * 512:(db + 1) * 512].rearrange("(c k) n -> k c n", k=128)), wb.ds, 1, writes=[wb.r])
            return wb, wvd

        xin_next = gather(0)
        for e_ in range(NE):
            xin = xin_next
            xT = xTR.next()
            for c in range(16):
                pt, ptr = psT.next()
                ptb = pt[:, :].bitcast(BF16)

                def tr(e, ptb=ptb, xin=xin, c=c):
                    for j, (a0, an) in enumerate(chunks):
                        q = e.transpose(out=ptb[:, a0:a0 + an], in_=xin.t[0:an, j, c * 128:(c + 1) * 128], identity=ident_b[0:an, 0:an])
                    return q
                p.op("pe", tr, reads=[xin.r, cst_r], writes=[ptr])
                p.op("act", lambda e, ptb=ptb, xT=xT, c=c: e.activation(out=xT.t[:, c, 0:CAP_L], in_=ptb[:, 0:CAP_L], func=AF.Identity,
                                                                     scale=Am[:, c, 0:1], bias=Bm[:, c, 0:1]),
                     reads=[ptr, am_r, modcol_r[l]], awrites=[xT.r])
                if need_ctx:
                    p.op("act", lambda e, ptb=ptb, xT=xT, c=c: e.activation(out=xT.t[:, c, CAP_L:CAPT], in_=ptb[:, CAP_L:CAPT], func=AF.Identity,
                                                                         scale=Am[:, c, 1:2], bias=Bm[:, c, 1:2]),
                         reads=[ptr, am_r, modcol_r[l]], awrites=[xT.r])
            if e_ + 1 < NE:
                xin_next = gather(e_ + 1)
            if MOE_STEPS < 3:
                continue
            hid = hidR.next()
            for jb in range(4):
                wb, wvg, wvu = wunit_gu(e_, jb)
                for fc in range(2):
                    pg, pgr = psG.next()
                    pu, pur = psU.next()

                    def fg(e, pg=pg, wvg=wvg, fc=fc, xT=xT):
                        for k in range(16):
                            q = e.matmul(pg[:, 0:ncap], lhsT=wvg[:, k, fc * 128:(fc + 1) * 128], rhs=xT.t[:, k, 0:ncap], start=(k == 0), stop=(k == 15))
                        return q

                    def fu(e, pu=pu, wvu=wvu, fc=fc, xT=xT):
                        for k in range(16):
                            q = e.matmul(pu[:, 0:ncap], lhsT=wvu[:, k, fc * 128:(fc + 1) * 128], rhs=xT.t[:, k, 0:ncap], start=(k == 0), stop=(k == 15))
                        return q
                    p.op("pe", fg, reads=[wb.r, xT.r], writes=[pgr])
                    p.op("pe", fu, reads=[wb.r, xT.r], writes=[pur])
                    sg = sgR.next()
                    p.op("act", lambda e, pg=pg, sg=sg: e.activation(out=sg.t[:, 0:ncap], in_=pg[:, 0:ncap], func=AF.Silu), reads=[pgr], writes=[sg.r])
                    uu = uuR.next()
                    p.op("dve", lambda e, pu=pu, uu=uu: e.tensor_copy(out=uu.t[:, 0:ncap], in_=pu[:, 0:ncap]), reads=[pur], writes=[uu.r])
                    f8 = jb * 2 + fc
                    p.op("pool", lambda e, uu=uu, sg=sg, hid=hid, f8=f8: e.tensor_tensor(out=hid.t[:, f8, 0:ncap], in0=uu.t[:, 0:ncap], in1=sg.t[:, 0:ncap], op=ALU.mult),
                         reads=[uu.r, sg.r], awrites=[hid.r])
            ost = ostR.next()
            for db in range(4):
                wb, wvd = wunit_d(e_, db)
                for j, (a0, an) in enumerate(chunks):
                    r = 0 if j < 2 else 1
                    pd, pdr = psD.next()

                    def fd(e, pd=pd, wvd=wvd, hid=hid, a0=a0, an=an):
                        for f in range(8):
                            q = e.matmul(pd[0:an, :], lhsT=hid.t[:, f, a0:a0 + an], rhs=wvd[:, f, :], start=(f == 0), stop=(f == 7))
                        return q
                    p.op("pe", fd, reads=[wb.r, hid.r], writes=[pdr])
                    to = toR.next()
                    p.op("act", lambda e, pd=pd, to=to, j=j, an=an, e_=e_: e.activation(out=to.t[0:an, :], in_=pd[0:an, :], func=AF.Copy, scale=gT[0:an, j, e_:e_ + 1]),
                         reads=[pdr, tk_r], writes=[to.r])
                    p.op("pool", lambda e, to=to, ost=ost, j=j, an=an, db=db, r=r: e.tensor_tensor(
                        out=ost.t[0:an, j, db * 512:(db + 1) * 512], in0=to.t[0:an, :], in1=g2b[0:an, r, db * 512:(db + 1) * 512], op=ALU.mult),
                        reads=[to.r, g2_r], awrites=[ost.r])
            for j, (a0, an) in enumerate(chunks):
                if MOE_STEPS < 4:
                    continue
                p.dma("pool", lambda e, ost=ost, j=j, an=an, e_=e_: [e.indirect_dma_start(
                    out=xres[:, :], out_offset=bass.IndirectOffsetOnAxis(ap=idxT[0:an, j, e_:e_ + 1], axis=0),
                    in_=ost.t[0:an, j, :], in_offset=None, compute_op=ALU.add)],
                    "scat", 1, reads=[ost.r, tk_r], writes=all_x)

    def phase_final(es):
        fnb = AS(es, "fnb", [128, D], F32)
        fn_r = Res("fnb")
        p.dma("sp", dma1(fnb[:], bcast_rows(I("final_norm")[0:1, :], 128)), "sm2", 1, writes=[fn_r])
        xtR = ringS(es, "fx", [128, D], F32, 3)
        oR = ringS(es, "fo", [128, D], F32, 2)
        junk = AS(es, "fjunk", [128, D], BF16)
        junk_r = Res("junkf")
        ssR = ringS(es, "fs", [128, 4], F32, 3)
        for i in range(16):
            xt = xtR.next()
            p.dma("sp", dma1(xt.t[:], xres[i * 128:(i + 1) * 128, :]), xt.ds, 1, reads=[xres_r[i]], writes=[xt.r])
            ss = ssR.next()
            p.op("act", lambda e, xt=xt, ss=ss: e.activation(out=junk[:], in_=xt.t[:], func=AF.Square, accum_out=ss.t[:, 0:1]),
                 reads=[xt.r], writes=[junk_r, ss.r])
            p.op("act", lambda e, ss=ss: e.activation(out=ss.t[:, 1:2], in_=ss.t[:, 0:1], func=AF.Sqrt, scale=1.0 / D, bias=EPS),
                 reads=[ss.r], writes=[ss.r])
            p.op("dve", lambda e, ss=ss: e.reciprocal(out=ss.t[:, 2:3], in_=ss.t[:, 1:2]), reads=[ss.r], writes=[ss.r])
            o = oR.next()
            p.op("dve", lambda e, xt=xt, ss=ss, o=o: e.scalar_tensor_tensor(out=o.t[:], in0=xt.t[:], scalar=ss.t[:, 2:3], in1=fnb[:], op0=ALU.mult, op1=ALU.mult),
                 reads=[xt.r, ss.r, fn_r], writes=[o.r])
            p.dma("sp", dma1(out[i * 128:(i + 1) * 128, :], o.t[:]), o.ds, 1, reads=[o.r], awrites=[out_r])

    for l in range(n_layers):
        is_ab = (l % 2 == 0)
        li = l // 2
        need_ctx = l < DEPTH - 1
        tiles_tok = list(range(NT))
        with ExitStack() as esL:
            big = AS(esL, "big", [128, 16, T], BF16)
            big_rs = [Res(f"big{c}") for c in range(16)]
            with ExitStack() as es1:
                phase1(es1, l, big, big_rs)
                p.barrier()
            if stop == ("p1", l):
                dbg = nc.dram_tensor("dbg_hT", [128, 16, T], BF16, kind="ExternalOutput").ap()
                dr = Res("dbg")
                p.dma("sp", dma1(dbg, big[:]), "dbg", 1, reads=big_rs, writes=[dr])
                return finish([dr])
            with ExitStack() as es2:
                if is_ab:
                    phase2_ab(es2, l, li, big, big_rs, tiles_tok)
                else:
                    phase2_cd(es2, l, li, big, big_rs, tiles_tok)
                p.barrier()
            if stop == ("p2", l):
                return finish()
            with ExitStack() as es3:
                if is_ab:
                    phase3_ab(es3, l, li, big, big_rs, need_ctx)
                else:
                    phase3_cd(es3, l, li, big, big_rs, need_ctx)
                p.barrier()
            if stop == ("p3", l):
                dbg = nc.dram_tensor("dbg_aoT", [128, 16, T], BF16, kind="ExternalOutput").ap()
                dr = Res("dbg")
                p.dma("sp", dma1(dbg, big[:]), "dbg", 1, reads=big_rs, writes=[dr])
                return finish([dr])
            with ExitStack() as es4:
                phase4(es4, l, (I("ab_w_out") if is_ab else I("cd_w_out"))[li], big, big_rs, need_ctx)
                p.barrier()
        if stop == ("p4", l):
            return finish()
        with ExitStack() as es5:
            phase5(es5, l, need_ctx)
            p.barrier()
        if stop == ("p5", l):
            return finish()
    with ExitStack() as esF:
        phase_final(esF)
    return finish()


_CONSTS = None


def _consts():
    global _CONSTS
    if _CONSTS is None:
        (C128, S128, P128), (C64, S64, P64) = ROPE128, ROPE64
        _CONSTS = {
            "c_rc128": _bf(C128), "c_rs128": _bf(S128), "c_rp128": _bf(P128),
            "c_rc64": _bf(C64), "c_rs64": _bf(S64), "c_rp64": _bf(P64),
            "c_band": _bf(BAND), "c_nbrm": _bf(NBR_MASKS.transpose(1, 0, 2)),
            "c_J": np.ascontiguousarray(np.kron(np.eye(2, dtype=np.float32), np.eye(64, dtype=np.float32)[::-1])),
        }
    return _CONSTS


def make_in_maps(inputs, n_cores=N_CORES, used=None):
    f = lambda a: np.ascontiguousarray(np.asarray(a, dtype=np.float32))
    shared = {k: f(inputs[k]) for k in ["w_ada", "b_ada", "norm1", "norm2", "ab_w_in", "ab_sink", "ab_q_norm", "ab_w_uq",
                                        "ab_kv_norm", "ab_w_ukv", "ab_w_out", "cd_w_in", "cd_q_norm", "cd_k_norm", "cd_rpb",
                                        "cd_w_out", "w_router", "w_gate", "w_up", "w_down"]}
    shared["final_norm"] = f(inputs["final_norm"]).reshape(1, D)
    shared.update(_consts())
    x = f(inputs["x"])
    c = f(inputs["c"])
    ctx = f(inputs["ctx"])
    c_ctx = f(inputs["c_ctx"])
    maps = []
    for i in range(n_cores):
        b = i % 4
        m = dict(shared)
        m["x"] = x[b]
        m["ctx"] = ctx[b]
        m["cvec"] = np.stack([c[b], c_ctx], 0)
        if used is not None:
            m = {k: v for k, v in m.items() if k in used}
        maps.append(m)
    return maps


def kernel(**inputs):
    nc = build()
    maps = make_in_maps(inputs, used=nc.used_inputs)
    res = run_bass_kernel_spmd(nc, maps, core_ids=list(range(N_CORES)))
    return np.stack([res.results[b]["out"] for b in range(4)], 0).astype(np.float32)
```

```python
import math
from contextlib import ExitStack

import numpy as np
import ml_dtypes
import concourse.bass as bass
import concourse.mybir as mybir
from concourse.bass_utils import run_bass_kernel_spmd

F32 = mybir.dt.float32
BF16 = mybir.dt.bfloat16
I32 = mybir.dt.int32
U32 = mybir.dt.uint32
AF = mybir.ActivationFunctionType
ALU = mybir.AluOpType

D = 2048
S = 2048
LC = 256
T = S + LC
NT = T // 128
DEPTH = 4
EPS = 1e-6
NE = 16
CAP_L = 256
CAP_C = 32
CAPT = CAP_L + CAP_C
DE = 1024
TBLK = [(0, 512), (512, 512), (1024, 512), (1536, 512), (2048, 256)]
N_CORES = 4
P2_LIMIT = 6
DBG_NOROPE = False
ROPE_STEPS = 9
ATT_LIMIT = 10 ** 9
ATT_COUNT = [0]
ATT_STEPS = 9
MOE_STEPS = 9
ROPE_ADD_ENG = 'pool'


class Res:
    __slots__ = ("name", "w", "r")

    def __init__(self, name=""):
        self.name = name
        self.w = {}
        self.r = {}


class DSem:
    def __init__(self, nc, name):
        self.sem = nc.alloc_semaphore(name)
        self.cum = 0


class Prog:
    ENG = ["sp", "act", "pool", "dve", "pe"]

    def __init__(self, nc):
        self.nc = nc
        self.ops = {e: [] for e in self.ENG}
        self.cnt = {e: 0 for e in self.ENG}
        self.esem = {e: nc.alloc_semaphore("es_" + e) for e in self.ENG}
        self.seen = {e: {} for e in self.ENG}
        self._dsems = {}
        self.ninst = 0

    def dsem(self, name):
        if name not in self._dsems:
            self._dsems[name] = DSem(self.nc, "ds_" + name)
        return self._dsems[name]

    def _collect(self, eng, reads, writes, awrites=(), extra=()):
        waits = {}
        own = self.esem[eng]

        def need(sem, val):
            if sem is own and eng == "pe":
                return
            if val > self.seen[eng].get(sem, 0) and val > waits.get(sem, 0):
                waits[sem] = val

        for r in reads:
            for s, v in r.w.items():
                need(s, v)
        for w in writes:
            for s, v in w.w.items():
                need(s, v)
            for s, v in w.r.items():
                need(s, v)
        for w in awrites:
            for s, v in w.r.items():
                need(s, v)
        for s, v in extra:
            need(s, v)
        for s, v in waits.items():
            self.seen[eng][s] = v
        return list(waits.items())

    def _mark(self, tok, reads, writes, awrites=()):
        s, v = tok
        for r in reads:
            if v > r.r.get(s, 0):
                r.r[s] = v
        for w in writes:
            w.w = {s: v}
            w.r = {}
        for w in awrites:
            if v > w.w.get(s, 0):
                w.w[s] = v

    def op(self, eng, fn, reads=(), writes=(), awrites=()):
        waits = self._collect(eng, reads, writes, awrites)
        self.cnt[eng] += 1
        tok = (self.esem[eng], self.cnt[eng])
        self.ops[eng].append((waits, fn, (self.esem[eng], 1)))
        self._mark(tok, reads, writes, awrites)
        return tok

    def dma(self, q, fn, dsem, n=1, reads=(), writes=(), awrites=()):
        if isinstance(dsem, str):
            dsem = self.dsem(dsem)
        waits = self._collect(q, reads, writes, awrites, extra=[(dsem.sem, dsem.cum)] if dsem.cum else [])
        dsem.cum += 16 * n
        tok = (dsem.sem, dsem.cum)
        self.ops[q].append((waits, fn, (dsem.sem, 16)))
        self._mark(tok, reads, writes, awrites)
        return tok

    def barrier(self):
        toks = [(self.esem[e], self.cnt[e]) for e in self.ENG if self.cnt[e] > 0]
        toks += [(d.sem, d.cum) for d in self._dsems.values() if d.cum]
        for e in self.ENG:
            waits = self._collect(e, (), (), extra=toks)
            if waits:
                self.ops[e].append((waits, None, None))

    def emit(self, final_tokens=()):
        nc = self.nc
        fw = {}
        for s, v in final_tokens:
            fw[s] = max(fw.get(s, 0), v)

        def run(name, e):
            for waits, fn, inc in self.ops[name]:
                for s, v in waits:
                    e.wait_ge(s, v)
                if fn is None:
                    continue
                r = fn(e)
                sem, k = inc
                if isinstance(r, (list, tuple)):
                    for ins in r:
                        ins.then_inc(sem, k)
                else:
                    r.then_inc(sem, k)
            if name == "sp":
                for s, v in fw.items():
                    e.wait_ge(s, v)

        with nc.Block() as block:
            @block.sync
            def _(e):
                run("sp", e)

            @block.scalar
            def _(e):
                run("act", e)

            @block.gpsimd
            def _(e):
                run("pool", e)

            @block.vector
            def _(e):
                run("dve", e)

            @block.tensor
            def _(e):
                run("pe", e)


class Slot:
    __slots__ = ("t", "r", "ds")

    def __init__(self, t, r, ds):
        self.t = t
        self.r = r
        self.ds = ds


class Ring:
    def __init__(self, alloc, name, shape, dtype, bufs):
        self.s = [Slot(alloc(f"{name}{i}", shape, dtype), Res(f"{name}{i}"), f"{name}{i}") for i in range(bufs)]
        self.i = 0

    def next(self):
        s = self.s[self.i % len(self.s)]
        self.i += 1
        return s


def bcast_rows(ap, n):
    return bass.AP(ap.tensor, ap.offset, [[0, n]] + [list(x) for x in ap.ap[1:]])


def _rope_tables():
    t = np.arange(S)
    rows, cols = t // 64, t % 64

    def tab(dh):
        half = dh // 2
        nf = half // 2
        inv = 10000.0 ** (-np.arange(0, half, 2, dtype=np.float32) / half)
        C = np.ones((dh, T), np.float32)
        Sg = np.zeros((dh, T), np.float32)
        for f in range(dh):
            pos = rows if f < half else cols
            ff = f % half
            fi = ff % nf
            ang = pos.astype(np.float32) * inv[fi]
            C[f, :S] = np.cos(ang)
            Sg[f, :S] = (-np.sin(ang)) if ff < nf else np.sin(ang)
        Pm = np.zeros((dh, dh), np.float32)
        for f in range(dh):
            ff = f % half
            partner = f + nf if ff < nf else f - nf
            Pm[partner, f] = 1.0
        return C, Sg, Pm

    return tab(128), tab(64)


def _band_mask():
    j = np.arange(128)[:, None]
    i = np.arange(384)[None, :]
    return (np.abs(i - 128 - j) <= 128).astype(np.float32)


def _nbr_plan():
    rows_n = 32
    kc = np.arange(64)[:, None]
    qc = np.arange(64)[None, :]
    cs = np.clip(qc - 8, 0, 48)
    colmask = ((kc >= cs) & (kc < cs + 16)).astype(np.float32)
    pats = {}
    plan = []
    for qt in range(16):
        rs = [int(np.clip(r - 4, 0, rows_n - 8)) for r in (2 * qt, 2 * qt + 1)]
        k_lo = min(rs) // 2
        k_hi = (max(rs) + 7) // 2
        ent = []
        for kt in range(k_lo, k_hi + 1):
            blk = []
            for kr in range(2):
                for qr in range(2):
                    krow = 2 * kt + kr
                    blk.append(1 if rs[qr] <= krow < rs[qr] + 8 else 0)
            key = tuple(blk)
            if sum(key) == 0:
                continue
            if key not in pats:
                m = np.zeros((128, 128), np.float32)
                for kr in range(2):
                    for qr in range(2):
                        if key[kr * 2 + qr]:
                            m[kr * 64:(kr + 1) * 64, qr * 64:(qr + 1) * 64] = colmask
                pats[key] = (len(pats), m)
            ent.append((kt, kt - qt, pats[key][0]))
        plan.append(ent)
    masks = np.stack([m for _, m in sorted(pats.values(), key=lambda x: x[0])])
    return plan, masks


ROPE128, ROPE64 = _rope_tables()
BAND = _band_mask()
NBR_PLAN, NBR_MASKS = _nbr_plan()
NPAT = NBR_MASKS.shape[0]


def _bf(a):
    return np.ascontiguousarray(a).astype(ml_dtypes.bfloat16)


INPUT_SHAPES = {
    "x": ([S, D], "f"), "ctx": ([LC, D], "f"), "cvec": ([2, D], "f"),
    "w_ada": ([DEPTH, D, 6 * D], "f"), "b_ada": ([DEPTH, 6 * D], "f"),
    "norm1": ([DEPTH, D], "f"), "norm2": ([DEPTH, D], "f"),
    "ab_w_in": ([2, D, 2368], "f"), "ab_sink": ([2, 8], "f"), "ab_q_norm": ([2, 512], "f"),
    "ab_w_uq": ([2, 512, 1536], "f"), "ab_kv_norm": ([2, 256], "f"), "ab_w_ukv": ([2, 256, 2048], "f"),
    "ab_w_out": ([2, D, D], "f"), "cd_w_in": ([2, D, 4608], "f"), "cd_q_norm": ([2, 128], "f"),
    "cd_k_norm": ([2, 128], "f"), "cd_rpb": ([2, 8, 15, 31], "f"), "cd_w_out": ([2, D, D], "f"),
    "w_router": ([DEPTH, D, NE], "f"), "w_gate": ([DEPTH, NE, D, DE], "f"), "w_up": ([DEPTH, NE, D, DE], "f"),
    "w_down": ([DEPTH, NE, DE, D], "f"), "final_norm": ([1, D], "f"),
    "c_rc128": ([128, T], "b"), "c_rs128": ([128, T], "b"), "c_rp128": ([128, 128], "b"),
    "c_rc64": ([64, T], "b"), "c_rs64": ([64, T], "b"), "c_rp64": ([64, 64], "b"),
    "c_band": ([128, 384], "b"), "c_nbrm": ([128, NPAT, 128], "b"), "c_J": ([128, 128], "f"),
}


def build(n_layers=DEPTH, debug=False, stop=None):
    nc = bass.Bass("TRN2", target_bir_lowering=False)
    p = Prog(nc)
    used = {}

    def I(name):
        if name not in used:
            shp, k = INPUT_SHAPES[name]
            used[name] = nc.dram_tensor(name, list(shp), F32 if k == "f" else BF16, kind="ExternalInput").ap()
        return used[name]

    nc.used_inputs = used

    def dscr(name, shape, dt):
        return nc.dram_tensor(name, list(shape), dt, kind="ExternalOutput" if debug else "Internal").ap()

    out = nc.dram_tensor("out", [S, D], F32, kind="ExternalOutput").ap()
    xres = dscr("xres", [T, D], F32)
    xn_d = dscr("xn_d", [T, D], BF16)
    modrow_d = dscr("modrow_d", [DEPTH, 2, 6 * D], F32)
    FM_d = dscr("FM_d", [4096, T], BF16)
    TM_d = dscr("TM_d", [T, 1280], BF16)
    rpbpad_d = dscr("rpbpad_d", [120, 160], F32)

    xres_r = [Res(f"xres{i}") for i in range(NT)]
    xn_r = Res("xn_d")
    modrow_r = Res("modrow")
    FM_r = Res("FM")
    TM_r = Res("TM")
    rpb_r = Res("rpbpad")
    out_r = Res("out")

    def A(name, shape, dt):
        return nc.alloc_sbuf_tensor(name, list(shape), dt)

    uid = [0]

    def AS(es, name, shape, dt):
        uid[0] += 1
        return es.enter_context(nc.sbuf_tensor(f"{name}_u{uid[0]}", list(shape), dt))

    def ringS(es, name, shape, dt, bufs):
        return Ring(lambda n, s, d: AS(es, n, s, d), name, shape, dt, bufs)

    ident_f = A("ident_f", [128, 128], F32)
    ident_b = A("ident_b", [128, 128], BF16)
    ones_b = A("ones_b", [128, 128], BF16)
    ones_f = A("ones_f", [1, 128], F32)
    rp128 = A("rp128", [128, 128], BF16)
    rp64 = A("rp64", [64, 64], BF16)
    cst_r = Res("consts")
    scT = A("scT", [128, 16, 2], BF16)
    ncol = A("ncol", [128, 8, 16], F32)
    small_r = Res("small")
    modcol = [A(f"modcol{l}", [128, 96, 2], F32) for l in range(DEPTH)]
    modcol_r = [Res(f"modcol{l}") for l in range(DEPTH)]
    affT = A("affT", [16, T], F32)
    affT_r = Res("affT")
    Wr = Ring(A, "W", [128, 8192], BF16, 4)

    psb = [nc.alloc_psum_tensor(f"ps{i}", [128, 512], F32) for i in range(8)]
    psr = [Res(f"ps{i}") for i in range(8)]

    class PsRing:
        def __init__(self, idxs):
            self.idxs = idxs
            self.i = 0

        def next(self):
            k = self.idxs[self.i % len(self.idxs)]
            self.i += 1
            return psb[k], psr[k]

    def dma1(out_ap, in_ap, **kw):
        return lambda e: [e.dma_start(out=out_ap, in_=in_ap, **kw)]

    NCG = dict(allow_slow_non_contiguous=True)

    def wload(src_ap, ncols, kch):
        wb = Wr.next()
        wv = wb.t[:, 0:kch * ncols].rearrange("p (c n) -> p c n", c=kch)
        p.dma("pool", dma1(wv, src_ap.rearrange("(c k) n -> k c n", k=128)), wb.ds, 1, writes=[wb.r])
        return wb, wv

    p.op("pool", lambda e: e.memset(ident_f[:], 0.0), writes=[cst_r])
    p.op("pool", lambda e: e.affine_select(out=ident_f[:], in_=ident_f[:], pattern=[[-1, 128]], compare_op=ALU.not_equal,
                                           fill=1.0, base=0, channel_multiplier=1), reads=[cst_r], writes=[cst_r])
    p.op("dve", lambda e: e.tensor_copy(out=ident_b[:], in_=ident_f[:]), reads=[cst_r], writes=[cst_r])
    p.op("dve", lambda e: e.memset(ones_b[:], 1.0), writes=[cst_r])
    p.op("dve", lambda e: e.memset(ones_f[:], 1.0), writes=[cst_r])
    for dst, src in [(rp128, "c_rp128"), (rp64, "c_rp64")]:
        p.dma("sp", dma1(dst[:], I(src)), "cst", 1, writes=[cst_r])

    with ExitStack() as es0:
        craw = AS(es0, "craw", [128, 16, 2], F32)
        for r in range(2):
            p.dma("sp", dma1(craw[:, :, r], I("cvec")[r].rearrange("(c p) -> p c", p=128), **NCG), "sm", 1, writes=[small_r])
        for l in range(DEPTH):
            p.dma("sp", dma1(ncol[:, l, :], I("norm1")[l].rearrange("(c p) -> p c", p=128), **NCG), "sm", 1, writes=[small_r])
            p.dma("sp", dma1(ncol[:, 4 + l, :], I("norm2")[l].rearrange("(c p) -> p c", p=128), **NCG), "sm", 1, writes=[small_r])
        p.op("act", lambda e: e.activation(out=scT[:], in_=craw[:], func=AF.Silu), reads=[small_r], writes=[small_r])

        rbR = ringS(es0, "rb", [2, 512], F32, 2)
        btR = ringS(es0, "bt", [2, 512], F32, 2)
        psM = PsRing([0, 1])
        psTm = PsRing([2, 3])
        for l in range(n_layers):
            for nb in range(24):
                wb, wv = wload(I("w_ada")[l, :, nb * 512:(nb + 1) * 512], 512, 16)
                bt = btR.next()
                p.dma("sp", dma1(bt.t[:], bcast_rows(I("b_ada")[l:l + 1, nb * 512:(nb + 1) * 512], 2)), bt.ds, 1, writes=[bt.r])
                ps, pr = psM.next()

                def mm(e, ps=ps, wv=wv):
                    for k in range(16):
                        r = e.matmul(ps[0:2, :], lhsT=scT[:, k, :], rhs=wv[:, k, :], start=(k == 0), stop=(k == 15))
                    return r
                p.op("pe", mm, reads=[wb.r, small_r], writes=[pr])
                rb = rbR.next()
                p.op("dve", lambda e, ps=ps, rb=rb, bt=bt: e.tensor_tensor(out=rb.t[:], in0=ps[0:2, :], in1=bt.t[:], op=ALU.add),
                     reads=[pr, bt.r], writes=[rb.r])
                p.dma("sp", dma1(modrow_d[l, :, nb * 512:(nb + 1) * 512], rb.t[:]), rb.ds, 1, reads=[rb.r], awrites=[modrow_r])
                pt, ptr = psTm.next()

                def tr(e, pt=pt, rb=rb):
                    for j in range(4):
                        r = e.transpose(out=pt[:, j * 2:(j + 1) * 2], in_=rb.t[0:2, j * 128:(j + 1) * 128], identity=ident_f[0:2, 0:2])
                    return r
                p.op("pe", tr, reads=[rb.r, cst_r], writes=[ptr])
                mcv = modcol[l][:, :, :].rearrange("p c r -> p (c r)")
                p.op("dve", lambda e, pt=pt, mcv=mcv, nb=nb: e.tensor_copy(out=mcv[:, nb * 8:(nb + 1) * 8], in_=pt[:, 0:8]),
                     reads=[ptr], writes=[modcol_r[l]])
        p.barrier()

    def finish(extra_res=()):
        toks = []
        for r in list(extra_res) + [out_r, modrow_r, FM_r, TM_r, xn_r] + xres_r:
            toks += list(r.w.items())
        p.barrier()
        p.emit(toks)
        return nc

    if stop == "mod":
        return finish()

    SQD = math.sqrt(D)

    def make_AB(es, l, which):
        Am = AS(es, f"Am{which}", [128, 16, 2], F32)
        r_ = Res(f"Am{which}")
        sc0 = 16 if which == 1 else 64
        sh0 = 0 if which == 1 else 48
        nrow = l if which == 1 else 4 + l
        p.op("dve", lambda e: e.tensor_scalar(out=Am[:], in0=modcol[l][:, sc0:sc0 + 16, :], scalar1=1.0, scalar2=SQD,
                                              op0=ALU.add, op1=ALU.mult), reads=[modcol_r[l]], writes=[r_])
        for r in range(2):
            p.op("dve", lambda e, r=r: e.tensor_tensor(out=Am[:, :, r], in0=Am[:, :, r], in1=ncol[:, nrow, :], op=ALU.mult),
                 reads=[r_, small_r], writes=[r_])
        Bm = modcol[l][:, sh0:sh0 + 16, :]
        return Am, Bm, r_

    def xsrc(l, i):
        if l == 0:
            return I("x")[i * 128:(i + 1) * 128, :] if i < 16 else I("ctx")[(i - 16) * 128:(i - 15) * 128, :]
        return xres[i * 128:(i + 1) * 128, :]

    def norm_tile(xt, ssR, junk, junk_r):
        ss = ssR.next()
        p.op("act", lambda e: e.activation(out=junk[:], in_=xt.t[:], func=AF.Square, accum_out=ss.t[:, 0:1]),
             reads=[xt.r], writes=[junk_r, ss.r])
        p.op("act", lambda e: e.activation(out=ss.t[:, 1:2], in_=ss.t[:, 0:1], func=AF.Sqrt, scale=1.0, bias=D * EPS),
             reads=[ss.r], writes=[ss.r])
        p.op("dve", lambda e: e.reciprocal(out=ss.t[:, 2:3], in_=ss.t[:, 1:2]), reads=[ss.r], writes=[ss.r])
        return ss

    def phase1(es, l, big, big_rs):
        Am, Bm, am_r = make_AB(es, l, 1)
        xtR = ringS(es, "p1x", [128, D], F32, 2)
        xnR = ringS(es, "p1n", [128, D], BF16, 5)
        junk = AS(es, "p1junk", [128, D], BF16)
        junk_r = Res("junk")
        ssR = ringS(es, "p1s", [128, 4], F32, 4)
        psT = PsRing([0, 1, 2, 3])
        ev = 0
        for (t0, tn) in TBLK:
            r = 0 if t0 < S else 1
            xns = []
            for ii in range(tn // 128):
                i = t0 // 128 + ii
                xt = xtR.next()
                p.dma("sp", dma1(xt.t[:], xsrc(l, i)), xt.ds, 1, reads=[xres_r[i]], writes=[xt.r])
                ss = norm_tile(xt, ssR, junk, junk_r)
                xn = xnR.next()
                p.op("act", lambda e, xt=xt, ss=ss, xn=xn: e.activation(out=xn.t[:], in_=xt.t[:], func=AF.Copy, scale=ss.t[:, 2:3]),
                     reads=[xt.r, ss.r], writes=[xn.r])
                xns.append(xn)
            for c in range(16):
                pt, ptr = psT.next()
                ptb = pt[:, :].bitcast(BF16)

                def tr(e, ptb=ptb, xns=xns, c=c):
                    for ii, xn in enumerate(xns):
                        r_ = e.transpose(out=ptb[:, ii * 128:(ii + 1) * 128], in_=xn.t[:, c * 128:(c + 1) * 128], identity=ident_b[:])
                    return r_
                p.op("pe", tr, reads=[x.r for x in xns] + [cst_r], writes=[ptr])
                dst = big[:, c, t0:t0 + tn]
                if ev % 2 == 0:
                    p.op("act", lambda e, ptb=ptb, dst=dst, c=c, r=r, tn=tn: e.activation(
                        out=dst, in_=ptb[:, 0:tn], func=AF.Identity, scale=Am[:, c, r:r + 1], bias=Bm[:, c, r:r + 1]),
                        reads=[ptr, am_r, modcol_r[l]], awrites=[big_rs[c]])
                else:
                    p.op("dve", lambda e, ptb=ptb, dst=dst, c=c, r=r, tn=tn: e.tensor_scalar(
                        out=dst, in0=ptb[:, 0:tn], scalar1=Am[:, c, r:r + 1], scalar2=Bm[:, c, r:r + 1], op0=ALU.mult, op1=ALU.add),
                        reads=[ptr, am_r, modcol_r[l]], awrites=[big_rs[c]])
                ev += 1

    class ProjCtx:
        pass

    def proj_setup(es):
        c = ProjCtx()
        c.qaR = ringS(es, "pqa", [128, 512], BF16, 2)
        c.t1R = ringS(es, "pt1", [128, 512], F32, 1)
        c.t2R = ringS(es, "pt2", [128, 512], F32, 1)
        c.osR = ringS(es, "pos", [128, 512], BF16, 3)
        c.sqR = ringS(es, "psq", [128, 512], BF16, 2)
        c.rbR = ringS(es, "prb", [128, 512], F32, 1)
        c.tmR = ringS(es, "ptm", [128, 512], F32, 1)
        c.rtR = {128: ringS(es, "prt", [128, 2, 512], BF16, 2), 64: ringS(es, "prt6", [64, 2, 512], BF16, 2)}
        c.rt_cur = {}
        c.psA = PsRing([0, 1, 2, 3])
        c.psP = PsRing([4, 5])
        c.psV = PsRing([6, 7])
        c.ev = 0
        return c

    def fm_matmul(c, lhs_fn, nk, rhs_fn, M, n, reads):
        ps, pr = c.psA.next()

        def f(e):
            for k in range(nk):
                r = e.matmul(ps[0:M, 0:n], lhsT=lhs_fn(k), rhs=rhs_fn(k), start=(k == 0), stop=(k == nk - 1))
            return r
        p.op("pe", f, reads=reads, writes=[pr])
        return ps, pr

    def store_fm(c, src_ap, src_reads, M, n, row0, t0, eng=None):
        o = c.osR.next()
        eng = eng or ("act" if c.ev % 2 == 0 else "dve")
        c.ev += 1
        if eng == "act":
            p.op("act", lambda e: e.activation(out=o.t[0:M, 0:n], in_=src_ap, func=AF.Copy), reads=src_reads, writes=[o.r])
        else:
            p.op(eng, lambda e: e.tensor_copy(out=o.t[0:M, 0:n], in_=src_ap), reads=src_reads, writes=[o.r])
        p.dma("sp", dma1(FM_d[row0:row0 + M, t0:t0 + n], o.t[0:M, 0:n]), o.ds, 1, reads=[o.r], awrites=[FM_r])

    def rope_tabs(c, which, t0, n):
        key = (which, t0)
        if c.rt_cur.get(which, (None, None))[0] != key:
            sl = c.rtR[which].next()
            cn, sn = ("c_rc128", "c_rs128") if which == 128 else ("c_rc64", "c_rs64")
            p.dma("sp", dma1(sl.t[:, 0, 0:n], I(cn)[:, t0:t0 + n]), sl.ds, 1, writes=[sl.r])
            p.dma("sp", dma1(sl.t[:, 1, 0:n], I(sn)[:, t0:t0 + n]), sl.ds, 1, awrites=[sl.r])
            c.rt_cur[which] = (key, sl)
        return c.rt_cur[which][1]

    def rope_store(c, src_ap, src_reads, M, n, t0, which, row0):
        if DBG_NOROPE:
            return store_fm(c, src_ap, src_reads, M, n, row0, t0)
        Pm = rp128 if which == 128 else rp64
        rt = rope_tabs(c, which, t0, n)
        if ROPE_STEPS == 0:
            return store_fm(c, src_ap, list(src_reads) + [rt.r], M, n, row0, t0)
        qa = c.qaR.next()
        p.op("act", lambda e: e.activation(out=qa.t[0:M, 0:n], in_=src_ap, func=AF.Copy), reads=src_reads, writes=[qa.r])
        if ROPE_STEPS == 1:
            return store_fm(c, src_ap, list(src_reads) + [rt.r, qa.r], M, n, row0, t0)
        t1 = c.t1R.next()
        p.op("dve", lambda e: e.tensor_tensor(out=t1.t[0:M, 0:n], in0=qa.t[0:M, 0:n], in1=rt.t[0:M, 0, 0:n], op=ALU.mult),
             reads=[qa.r, rt.r], writes=[t1.r])
        if ROPE_STEPS == 2:
            return store_fm(c, t1.t[0:M, 0:n], [t1.r, qa.r], M, n, row0, t0)
        ps2, pr2 = c.psP.next()
        p.op("pe", lambda e: e.matmul(ps2[0:M, 0:n], lhsT=Pm[0:M, 0:M], rhs=qa.t[0:M, 0:n], start=True, stop=True),
             reads=[qa.r, cst_r], writes=[pr2])
        if ROPE_STEPS == 3:
            return store_fm(c, ps2[0:M, 0:n], [t1.r, pr2], M, n, row0, t0)
        t2 = c.t2R.next()
        p.op("dve", lambda e: e.scalar_tensor_tensor(out=t2.t[0:M, 0:n], in0=ps2[0:M, 0:n], scalar=1.0, in1=rt.t[0:M, 1, 0:n], op0=ALU.mult, op1=ALU.mult),
             reads=[pr2, rt.r], writes=[t2.r])
        o = c.osR.next()
        p.op(ROPE_ADD_ENG, lambda e: e.tensor_tensor(out=o.t[0:M, 0:n], in0=t1.t[0:M, 0:n], in1=t2.t[0:M, 0:n], op=ALU.add),
             reads=[t1.r, t2.r], writes=[o.r])
        p.dma("sp", dma1(FM_d[row0:row0 + M, t0:t0 + n], o.t[0:M, 0:n]), o.ds, 1, reads=[o.r], awrites=[FM_r])

    def fm_rmsnorm(c, pss, nfeat, n, gain_cols, dsts, dst_res_list):
        pz, pzr = c.psP.next()
        npss = len(pss)
        for j, (ps, pr) in enumerate(pss):
            sq = c.sqR.next()
            p.op("act", lambda e, ps=ps, sq=sq: e.activation(out=sq.t[:, 0:n], in_=ps[:, 0:n], func=AF.Square), reads=[pr], writes=[sq.r])
            p.op("pe", lambda e, sq=sq, j=j: e.matmul(pz[:, 0:n], lhsT=ones_b[:], rhs=sq.t[:, 0:n], start=(j == 0), stop=(j == npss - 1)),
                 reads=[sq.r, cst_r], writes=[pzr])
        tm = c.tmR.next()
        p.op("act", lambda e: e.activation(out=tm.t[:, 0:n], in_=pz[:, 0:n], func=AF.Sqrt, scale=1.0 / nfeat, bias=EPS), reads=[pzr], writes=[tm.r])
        rb = c.rbR.next()
        p.op("dve", lambda e: e.reciprocal(out=rb.t[:, 0:n], in_=tm.t[:, 0:n]), reads=[tm.r], writes=[rb.r])
        for j, (ps, pr) in enumerate(pss):
            p.op("dve", lambda e, ps=ps, j=j: e.scalar_tensor_tensor(out=dsts[j], in0=ps[:, 0:n], scalar=gain_cols[j], in1=rb.t[:, 0:n],
                                                                    op0=ALU.mult, op1=ALU.mult),
                 reads=[pr, rb.r, small_r], awrites=[dst_res_list[j]])

    def tm_proj(c, lhs_src, lhs_reads, nk, rhs_fn, rhs_reads, ncols, col0, tiles):
        for i in tiles:
            ps, pr = c.psV.next()

            def f(e, ps=ps, i=i):
                for k in range(nk):
                    r = e.matmul(ps[:, 0:ncols], lhsT=lhs_src[:, k, i * 128:(i + 1) * 128], rhs=rhs_fn(k), start=(k == 0), stop=(k == nk - 1))
                return r
            p.op("pe", f, reads=list(lhs_reads) + list(rhs_reads), writes=[pr])
            o = c.osR.next()
            eng = "act" if c.ev % 2 == 0 else "dve"
            c.ev += 1
            if eng == "act":
                p.op("act", lambda e, ps=ps, o=o: e.activation(out=o.t[:, 0:ncols], in_=ps[:, 0:ncols], func=AF.Copy), reads=[pr], writes=[o.r])
            else:
                p.op("dve", lambda e, ps=ps, o=o: e.tensor_copy(out=o.t[:, 0:ncols], in_=ps[:, 0:ncols]), reads=[pr], writes=[o.r])
            p.dma("sp", dma1(TM_d[i * 128:(i + 1) * 128, col0:col0 + ncols], o.t[:, 0:ncols]), o.ds, 1, reads=[o.r], awrites=[TM_r])

    def col_load(dst_ap, src_1d, eng="sp"):
        p.dma(eng, dma1(dst_ap, src_1d.rearrange("(c p) -> p c", p=128), **NCG), "sm", 1, writes=[small_r])

    def phase2_ab(es, l, li, big, big_rs, tiles_tok):
        c = proj_setup(es)
        w_in = I("ab_w_in")[li]
        cqnT = AS(es, "cqnT", [128, 4, T], BF16)
        ckvnT = AS(es, "ckvnT", [128, 2, T], BF16)
        cq_rs = [Res(f"cqn{j}") for j in range(4)]
        ckv_rs = [Res(f"ckvn{j}") for j in range(2)]
        gq = AS(es, "gq", [128, 8], F32)
        col_load(gq[:, 0:4], I("ab_q_norm")[li])
        col_load(gq[:, 4:6], I("ab_kv_norm")[li])
        tblks = [tb for tb in TBLK if tb[0] < S or len(tiles_tok) > 16]
        chunks = [(0, 512), (512, 512), (1024, 512), (1536, 512), (2048, 320)]
        for ci, (c0, cn) in enumerate(chunks[:P2_LIMIT]):
            wb, wv = wload(w_in[:, c0:c0 + cn], cn, 16)
            rd = big_rs + [wb.r]
            for (t0, tn) in tblks:
                rhs = lambda k, t0=t0, tn=tn: big[:, k, t0:t0 + tn]
                if ci in (0, 1):
                    for m in range(4):
                        h = ci * 4 + m
                        ps, pr = fm_matmul(c, lambda k, m=m, wv=wv: wv[:, k, m * 128:(m + 1) * 128], 16, rhs, 128, tn, rd)
                        rope_store(c, ps[:, 0:tn], [pr], 128, tn, t0, 128, h * 128)
                elif ci == 2:
                    for m in range(2):
                        ps, pr = fm_matmul(c, lambda k, m=m, wv=wv: wv[:, k, m * 128:(m + 1) * 128], 16, rhs, 128, tn, rd)
                        rope_store(c, ps[:, 0:tn], [pr], 128, tn, t0, 128, 1024 + m * 128)
                elif ci == 3:
                    pss = [fm_matmul(c, lambda k, m=m, wv=wv: wv[:, k, m * 128:(m + 1) * 128], 16, rhs, 128, tn, rd) for m in range(4)]
                    fm_rmsnorm(c, pss, 512, tn, [gq[:, j:j + 1] for j in range(4)], [cqnT[:, j, t0:t0 + tn] for j in range(4)], cq_rs)
                else:
                    pss = [fm_matmul(c, lambda k, m=m, wv=wv: wv[:, k, m * 128:(m + 1) * 128], 16, rhs, 128, tn, rd) for m in range(2)]
                    fm_rmsnorm(c, pss, 256, tn, [gq[:, 4 + j:5 + j] for j in range(2)], [ckvnT[:, j, t0:t0 + tn] for j in range(2)], ckv_rs)
                    ps, pr = fm_matmul(c, lambda k, wv=wv: wv[:, k, 256:320], 16, rhs, 64, tn, rd)
                    rope_store(c, ps[0:64, 0:tn], [pr], 64, tn, t0, 64, 3840)
            if ci == 2:
                tm_proj(c, big, big_rs, 16, lambda k, wv=wv: wv[:, k, 256:512], [wb.r], 256, 0, tiles_tok)
        if P2_LIMIT < 6:
            return
        wb, wv = wload(I("ab_w_uq")[li], 1536, 4)
        for h in range(8):
            for (t0, tn) in tblks:
                rhs = lambda k, t0=t0, tn=tn: cqnT[:, k, t0:t0 + tn]
                ps, pr = fm_matmul(c, lambda k, h=h, wv=wv: wv[:, k, h * 192:h * 192 + 128], 4, rhs, 128, tn, cq_rs + [wb.r])
                store_fm(c, ps[:, 0:tn], [pr], 128, tn, 1280 + h * 128, t0)
                ps, pr = fm_matmul(c, lambda k, h=h, wv=wv: wv[:, k, h * 192 + 128:h * 192 + 192], 4, rhs, 64, tn, cq_rs + [wb.r])
                rope_store(c, ps[0:64, 0:tn], [pr], 64, tn, t0, 64, 2304 + h * 64)
        wb, wv = wload(I("ab_w_ukv")[li], 2048, 2)
        wv4 = wb.t[:, 0:4096].rearrange("p (c h x) -> p c h x", c=2, h=8)
        for h in range(8):
            for (t0, tn) in tblks:
                rhs = lambda k, t0=t0, tn=tn: ckvnT[:, k, t0:t0 + tn]
                ps, pr = fm_matmul(c, lambda k, h=h, wv=wv: wv[:, k, h * 256:h * 256 + 128], 2, rhs, 128, tn, ckv_rs + [wb.r])
                store_fm(c, ps[:, 0:tn], [pr], 128, tn, 2816 + h * 128, t0)
        for half in range(2):
            tm_proj(c, ckvnT, ckv_rs, 2, lambda k, half=half, wv4=wv4: wv4[:, k, half * 4:half * 4 + 4, 128:256], [wb.r], 512, 256 + half * 512, tiles_tok)

    class AttCtx:
        pass

    def att_setup(es, mla=True):
        a = AttCtx()
        a.psS = PsRing([0, 1, 2])
        a.psO = PsRing([3, 4])
        a.psZ = PsRing([5, 6])
        a.ptR = ringS(es, "apt", [128, 512], BF16, 4)
        a.rsR = ringS(es, "ars", [128, 512], F32, 1)
        a.r2R = ringS(es, "ar2", [128, 512], F32, 1)
        a.obR = ringS(es, "aob", [128, 512], F32, 1)
        a.qR = ringS(es, "aq", [128, T], BF16, 2)
        if mla:
            a.q2R = ringS(es, "aq2", [64, T], BF16, 2)
        a.kR = ringS(es, "ak", [128, T], BF16, 2)
        if mla:
            a.k2R = ringS(es, "ak2", [64, T], BF16, 1)
        a.vR = ringS(es, "av", [128, NT, 128], BF16, 2)
        a.band = AS(es, "band", [128, 384], BF16)
        a.nbrm = AS(es, "nbrm", [128, NPAT, 128], BF16)
        a.c_r = Res("attc")
        p.dma("sp", dma1(a.band[:], I("c_band")), "sm2", 1, writes=[a.c_r])
        p.dma("sp", dma1(a.nbrm[:], I("c_nbrm")), "sm2", 1, awrites=[a.c_r])
        return a

    def att_load_fm(ring, row0, M):
        s = ring.next()
        p.dma("sp", dma1(s.t[0:M, :], FM_d[row0:row0 + M, :]), s.ds, 1, reads=[FM_r], writes=[s.r])
        return s

    def att_load_v(a, col0):
        s = a.vR.next()
        p.dma("sp", dma1(s.t[:], TM_d[:, col0:col0 + 128].rearrange("(i p) v -> p i v", p=128)), s.ds, 1, reads=[TM_r], writes=[s.r])
        return s

    def attend(a, qparts, kparts, qk_reads, vs, q0, qn, tiles, scale, dst, dst_res, sink_ap=None, sink_reads=()):
        ATT_COUNT[0] += 1
        if ATT_COUNT[0] > ATT_LIMIT:
            return
        po, por = a.psO.next()
        pz, pzr = a.psZ.next()
        nt = len(tiles)
        sT = [None] * nt
        npart = len(qparts)

        def qk(i):
            kt, qa, qb, E, Er = tiles[i]
            ps, pr = a.psS.next()

            def f(e):
                for j in range(npart):
                    M = qparts[j][1]
                    r = e.matmul(ps[:, 0:qb - qa], lhsT=kparts[j][0][0:M, kt * 128:(kt + 1) * 128],
                                 rhs=qparts[j][0][0:M, q0 + qa:q0 + qb], start=(j == 0), stop=(j == npart - 1))
                return r
            p.op("pe", f, reads=qk_reads, writes=[pr])
            sT[i] = (ps, pr)

        PIPE = 2
        for i in range(min(PIPE, nt)):
            qk(i)
        for i in range(nt):
            kt, qa, qb, E, Er = tiles[i]
            n = qb - qa
            ps, pr = sT[i]
            pt = a.ptR.next()
            if ATT_STEPS < 2:
                if i + PIPE < nt:
                    qk(i + PIPE)
                continue
            p.op("act", lambda e, ps=ps, pt=pt, n=n: e.activation(out=pt.t[:, 0:n], in_=ps[:, 0:n], func=AF.Exp, scale=scale),
                 reads=[pr], writes=[pt.r])
            if E is not None and ATT_STEPS >= 3:
                p.op("dve", lambda e, pt=pt, n=n, E=E: e.tensor_tensor(out=pt.t[:, 0:n], in0=pt.t[:, 0:n], in1=E, op=ALU.mult),
                     reads=[pt.r] + list(Er), writes=[pt.r])
            if i + PIPE < nt:
                qk(i + PIPE)
            if ATT_STEPS < 4:
                continue

            def pv(e, pt=pt, kt=kt, qa=qa, qb=qb, n=n, i=i):
                e.matmul(po[:, qa:qb], lhsT=vs.t[:, kt, :], rhs=pt.t[:, 0:n], start=(i == 0), stop=(i == nt - 1))
                return e.matmul(pz[:, qa:qb], lhsT=ones_b[:], rhs=pt.t[:, 0:n], start=(i == 0), stop=(i == nt - 1))
            p.op("pe", pv, reads=[pt.r, vs.r, cst_r], writes=[por, pzr])
        if ATT_STEPS < 5:
            return
        rs = a.rsR.next()
        if sink_ap is not None:
            p.op("act", lambda e: e.activation(out=rs.t[:, 0:qn], in_=pz[:, 0:qn], func=AF.Identity, bias=sink_ap, scale=1.0),
                 reads=[pzr] + list(sink_reads), writes=[rs.r])
        else:
            p.op("act", lambda e: e.activation(out=rs.t[:, 0:qn], in_=pz[:, 0:qn], func=AF.Copy), reads=[pzr], writes=[rs.r])
        r2 = a.r2R.next()
        p.op("dve", lambda e: e.reciprocal(out=r2.t[:, 0:qn], in_=rs.t[:, 0:qn]), reads=[rs.r], writes=[r2.r])
        ob = a.obR.next()
        p.op("act", lambda e: e.activation(out=ob.t[:, 0:qn], in_=po[:, 0:qn], func=AF.Copy), reads=[por], writes=[ob.r])
        p.op("dve", lambda e: e.tensor_tensor(out=dst, in0=ob.t[:, 0:qn], in1=r2.t[:, 0:qn], op=ALU.mult),
             reads=[ob.r, r2.r], awrites=[dst_res])

    def qblocks(need_ctx):
        return [tb for tb in TBLK if tb[0] < S or need_ctx]

    CTX_TILES = [(16, None, None), (17, None, None)]

    def phase3_ab(es, l, li, big, big_rs, need_ctx):
        a = att_setup(es)
        snk = AS(es, "snk", [128, 8], F32)
        snk_r = Res("snk")
        p.dma("sp", dma1(snk[:], bcast_rows(I("ab_sink")[li:li + 1, :], 128)), "sm2", 1, writes=[snk_r])
        p.op("act", lambda e: e.activation(out=snk[:], in_=snk[:], func=AF.Exp), reads=[snk_r], writes=[snk_r])
        sc_a = 128 ** -0.5
        sc_b = 192 ** -0.5
        for kv in range(2):
            ks = att_load_fm(a.kR, 1024 + kv * 128, 128)
            vs = att_load_v(a, kv * 128)
            for g in range(4):
                h = kv * 4 + g
                qs = att_load_fm(a.qR, h * 128, 128)
                for (q0, qn) in qblocks(need_ctx):
                    tiles = [(16, 0, qn, None, ()), (17, 0, qn, None, ())]
                    if q0 < S:
                        for kt in range(max(0, q0 // 128 - 1), min(15, (q0 + qn) // 128) + 1):
                            lo = max(q0, kt * 128 - 128)
                            hi = min(q0 + qn, kt * 128 + 256)
                            if hi <= lo:
                                continue
                            m0 = lo - (kt * 128 - 128)
                            tiles.append((kt, lo - q0, hi - q0, a.band[:, m0:m0 + hi - lo], (a.c_r,)))
                    attend(a, [(qs.t, 128)], [(ks.t, 128)], [qs.r, ks.r], vs, q0, qn, tiles, sc_a,
                           big[:, h, q0:q0 + qn], big_rs[h], sink_ap=snk[:, h:h + 1], sink_reads=(snk_r,))
        kr = att_load_fm(a.k2R, 3840, 64)
        for h in range(8):
            ks = att_load_fm(a.kR, 2816 + h * 128, 128)
            vs = att_load_v(a, 256 + h * 128)
            qs = att_load_fm(a.qR, 1280 + h * 128, 128)
            q2 = att_load_fm(a.q2R, 2304 + h * 64, 64)
            for (q0, qn) in qblocks(need_ctx):
                kts = list(range(16, 18)) + (list(range(16)) if q0 < S else [])
                tiles = [(kt, 0, qn, None, ()) for kt in kts]
                attend(a, [(qs.t, 128), (q2.t, 64)], [(ks.t, 128), (kr.t, 64)], [qs.r, q2.r, ks.r, kr.r], vs, q0, qn, tiles, sc_b,
                       big[:, 8 + h, q0:q0 + qn], big_rs[8 + h])

    def phase2_cd(es, l, li, big, big_rs, tiles_tok):
        c = proj_setup(es)
        c.qnR = ringS(es, "pqn", [128, 512], F32, 2)
        w_in = I("cd_w_in")[li]
        gq = AS(es, "gqc", [128, 2], F32)
        col_load(gq[:, 0:1], I("cd_q_norm")[li])
        col_load(gq[:, 1:2], I("cd_k_norm")[li])
        tblks = [tb for tb in TBLK if tb[0] < S or len(tiles_tok) > 16]
        for ci in range(9):
            wb, wv = wload(w_in[:, ci * 512:(ci + 1) * 512], 512, 16)
            rd = big_rs + [wb.r]
            if ci in (7, 8):
                tm_proj(c, big, big_rs, 16, lambda k, wv=wv: wv[:, k, :], [wb.r], 512, 256 + (ci - 7) * 512, tiles_tok)
                continue
            for (t0, tn) in tblks:
                rhs = lambda k, t0=t0, tn=tn: big[:, k, t0:t0 + tn]
                nm = 2 if ci == 2 else 4
                for m in range(nm):
                    ps, pr = fm_matmul(c, lambda k, m=m, wv=wv: wv[:, k, m * 128:(m + 1) * 128], 16, rhs, 128, tn, rd)
                    if ci in (0, 1, 2):
                        row0 = (ci * 4 + m) * 128 if ci < 2 else 1024 + m * 128
                        g = gq[:, 0:1] if ci < 2 else gq[:, 1:2]
                        qn = c.qnR.next()
                        fm_rmsnorm(c, [(ps, pr)], 128, tn, [g], [qn.t[:, 0:tn]], [qn.r])
                        rope_store(c, qn.t[:, 0:tn], [qn.r], 128, tn, t0, 128, row0)
                    elif ci in (3, 4):
                        store_fm(c, ps[:, 0:tn], [pr], 128, tn, 1280 + ((ci - 3) * 4 + m) * 128, t0)
                    else:
                        store_fm(c, ps[:, 0:tn], [pr], 128, tn, 2304 + ((ci - 5) * 4 + m) * 128, t0)
            if ci == 2:
                tm_proj(c, big, big_rs, 16, lambda k, wv=wv: wv[:, k, 256:512], [wb.r], 256, 0, tiles_tok)

    def phase3_cd(es, l, li, big, big_rs, need_ctx):
        a = att_setup(es, mla=False)
        sc = 128 ** -0.5
        for kv in range(2):
            ks = att_load_fm(a.kR, 1024 + kv * 128, 128)
            vs = att_load_v(a, kv * 128)
            for g in range(4):
                h = kv * 4 + g
                qs = att_load_fm(a.qR, h * 128, 128)
                for (q0, qn) in qblocks(need_ctx):
                    kts = list(range(16, 18)) + (list(range(16)) if q0 < S else [])
                    tiles = [(kt, 0, qn, None, ()) for kt in kts]
                    attend(a, [(qs.t, 128)], [(ks.t, 128)], [qs.r, ks.r], vs, q0, qn, tiles, sc, big[:, h, q0:q0 + qn], big_rs[h])
        Jm = AS(es, "Jm", [128, 128], F32)
        padt = AS(es, "padt", [120, 160], F32)
        pad_r = Res("padt")
        p.dma("sp", dma1(Jm[:], I("c_J")), "sm2", 1, writes=[a.c_r])
        p.op("dve", lambda e: e.memset(padt[:], 0.0), writes=[pad_r])
        p.dma("sp", dma1(padt[:, 64:95], I("cd_rpb")[li].rearrange("h r c -> (h r) c")), "sm2", 1, writes=[pad_r])
        p.dma("sp", dma1(rpbpad_d[:, :], padt[:]), "sm2", 1, reads=[pad_r], writes=[rpb_r])
        keys = sorted({(d_, pt_) for ent in NBR_PLAN for (_, d_, pt_) in ent})
        deltas = sorted({d_ for d_, _ in keys})
        kidx = {k_: i for i, k_ in enumerate(keys)}
        egR = ringS(es, "eg", [128, len(keys), 128], BF16, 2)
        tTR = ringS(es, "egT", [128, 128], F32, 2)
        exR = ringS(es, "egx", [128, 128], F32, 2)
        pe_ps, pe_pr = psb[7], psr[7]
        for h in range(8):
            eg = egR.next()
            first = True
            for d_ in deltas:
                tT = tTR.next()
                nd = 0
                for qr in range(2):
                    for kr in range(2):
                        dr = min(14, max(0, 2 * d_ + kr - qr + 7))
                        src = bass.AP(rpbpad_d.tensor, (h * 15 + dr) * 160 + 16, [[1, 64], [1, 64]])
                        kw = dict(writes=[tT.r]) if nd == 0 else dict(awrites=[tT.r])
                        p.dma("sp", dma1(tT.t[qr * 64:(qr + 1) * 64, kr * 64:(kr + 1) * 64], src), tT.ds, 1, reads=[rpb_r], **kw)
                        nd += 1
                p.op("pe", lambda e, tT=tT: e.matmul(pe_ps[:, 0:128], lhsT=tT.t[:], rhs=Jm[:], start=True, stop=True),
                     reads=[tT.r, a.c_r], writes=[pe_pr])
                ex = exR.next()
                p.op("act", lambda e, ex=ex: e.activation(out=ex.t[:], in_=pe_ps[:, 0:128], func=AF.Exp), reads=[pe_pr], writes=[ex.r])
                for (dd, pt_) in keys:
                    if dd != d_:
                        continue
                    ki = kidx[(dd, pt_)]
                    kw = dict(writes=[eg.r]) if first else dict(awrites=[eg.r])
                    first = False
                    p.op("dve", lambda e, ex=ex, eg=eg, ki=ki, pt_=pt_: e.tensor_tensor(out=eg.t[:, ki, :], in0=ex.t[:], in1=a.nbrm[:, pt_, :], op=ALU.mult),
                         reads=[ex.r, a.c_r], **kw)
            ks = att_load_fm(a.kR, 2304 + h * 128, 128)
            vs = att_load_v(a, 256 + h * 128)
            qs = att_load_fm(a.qR, 1280 + h * 128, 128)
            for qt in range(16):
                tiles = [(16, 0, 128, None, ()), (17, 0, 128, None, ())]
                for (kt, d_, pt_) in NBR_PLAN[qt]:
                    tiles.append((kt, 0, 128, eg.t[:, kidx[(d_, pt_)], :], (eg.r,)))
                attend(a, [(qs.t, 128)], [(ks.t, 128)], [qs.r, ks.r], vs, qt * 128, 128, tiles, sc, big[:, 8 + h, qt * 128:(qt + 1) * 128], big_rs[8 + h])
            if need_ctx:
                tiles = [(16, 0, LC, None, ()), (17, 0, LC, None, ())]
                attend(a, [(qs.t, 128)], [(ks.t, 128)], [qs.r, ks.r], vs, S, LC, tiles, sc, big[:, 8 + h, S:T], big_rs[8 + h])

    def phase4(es, l, w_out, big, big_rs, need_ctx):
        ntile = NT if need_ctx else 16
        nr = 2 if need_ctx else 1
        wch = [wload(w_out[:, n * 512:(n + 1) * 512], 512, 16) for n in range(4)]
        g1b = AS(es, "g1b", [128, D], F32)
        g1_r = Res("g1b")
        Am, Bm, am_r = make_AB(es, l, 2)
        wr_sb = AS(es, "wr_sb", [128, 16, NE], F32)
        wrA = AS(es, "wrA", [128, 2, 16, NE], F32)
        biasr = AS(es, "biasr", [1, 2, NE], F32)
        biasb = AS(es, "biasb", [128, 2, NE], F32)
        wr_r = Res("wr")
        p.dma("sp", dma1(wr_sb[:], I("w_router")[l].rearrange("(c k) e -> k c e", k=128)), "sm2", 1, writes=[wr_r])
        for r in range(nr):
            for c in range(16):
                p.op("dve", lambda e, r=r, c=c: e.tensor_scalar(out=wrA[:, r, c, :], in0=wr_sb[:, c, :], scalar1=Am[:, c, r:r + 1], scalar2=None, op0=ALU.mult),
                     reads=[wr_r, am_r], awrites=[wr_r])
            pb, pbr = psb[7], psr[7]

            def fb(e, r=r):
                for c in range(16):
                    q = e.matmul(pb[0:1, 0:NE], lhsT=Bm[:, c, r:r + 1], rhs=wr_sb[:, c, :], start=(c == 0), stop=(c == 15))
                return q
            p.op("pe", fb, reads=[wr_r, modcol_r[l]], writes=[pbr])
            p.op("dve", lambda e, r=r: e.tensor_copy(out=biasr[0:1, r, :], in_=pb[0:1, 0:NE]), reads=[pbr], awrites=[wr_r])
            p.op("pe", lambda e, r=r: e.matmul(pb[:, 0:NE], lhsT=ones_f[0:1, :], rhs=biasr[0:1, r, :], start=True, stop=True),
                 reads=[wr_r, cst_r], writes=[pbr])
            p.op("dve", lambda e, r=r: e.tensor_copy(out=biasb[:, r, :], in_=pb[:, 0:NE]), reads=[pbr], awrites=[wr_r])
        xtR = ringS(es, "p4x", [128, D], F32, 2)
        tR = ringS(es, "p4t", [128, 512], F32, 2)
        xnbR = ringS(es, "p4nb", [128, D], BF16, 2)
        xTR = ringS(es, "p4xT", [128, 16, 128], F32, 1)
        ssR = ringS(es, "p4s", [128, 4], F32, 3)
        smR = ringS(es, "p4sm", [128, 40], F32, 2)
        psA = PsRing([0, 1, 2, 3])
        psT = PsRing([4, 5])
        for i in range(ntile):
            r = 0 if i < 16 else 1
            if i == 0 or i == 16:
                p.dma("sp", dma1(g1b[:], bcast_rows(modrow_d[l, r:r + 1, 2 * D:3 * D], 128)), "sm2", 1, reads=[modrow_r], writes=[g1_r])
            xt = xtR.next()
            p.dma("sp", dma1(xt.t[:], xsrc(l, i)), xt.ds, 1, reads=[xres_r[i]], writes=[xt.r])
            for n in range(4):
                wb, wv = wch[n]
                ps, pr = psA.next()

                def f(e, ps=ps, wv=wv, i=i):
                    for k in range(16):
                        q = e.matmul(ps[:, :], lhsT=big[:, k, i * 128:(i + 1) * 128], rhs=wv[:, k, :], start=(k == 0), stop=(k == 15))
                    return q
                p.op("pe", f, reads=big_rs + [wb.r], writes=[pr])
                t = tR.next()
                p.op("dve", lambda e, ps=ps, t=t, n=n: e.scalar_tensor_tensor(out=t.t[:], in0=ps[:, :], scalar=1.0, in1=g1b[:, n * 512:(n + 1) * 512], op0=ALU.mult, op1=ALU.mult),
                     reads=[pr, g1_r], writes=[t.r])
                p.op("pool", lambda e, xt=xt, t=t, n=n: e.tensor_tensor(out=xt.t[:, n * 512:(n + 1) * 512], in0=xt.t[:, n * 512:(n + 1) * 512], in1=t.t[:], op=ALU.add),
                     reads=[t.r, xt.r], writes=[xt.r])
            p.dma("sp", dma1(xres[i * 128:(i + 1) * 128, :], xt.t[:]), xt.ds, 1, reads=[xt.r], writes=[xres_r[i]])
            xnb = xnbR.next()
            ss = norm_tile(xt, ssR, xnb.t, xnb.r)
            p.op("act", lambda e, xt=xt, ss=ss, xnb=xnb: e.activation(out=xnb.t[:], in_=xt.t[:], func=AF.Copy, scale=ss.t[:, 2:3]),
                 reads=[xt.r, ss.r], writes=[xnb.r])
            p.dma("sp", dma1(xn_d[i * 128:(i + 1) * 128, :], xnb.t[:]), xnb.ds, 1, reads=[xnb.r], awrites=[xn_r])
            xT = xTR.next()
            for g4 in range(4):
                pt, ptr = psT.next()

                def tr(e, pt=pt, xt=xt, g4=g4):
                    for j in range(4):
                        cc = g4 * 4 + j
                        q = e.transpose(out=pt[:, j * 128:(j + 1) * 128], in_=xt.t[:, cc * 128:(cc + 1) * 128], identity=ident_f[:])
                    return q
                p.op("pe", tr, reads=[xt.r, cst_r], writes=[ptr])
                dstT = xT.t[:, g4 * 4:(g4 + 1) * 4, :].rearrange("p c t -> p (c t)")
                if g4 % 2 == 0:
                    p.op("act", lambda e, pt=pt, dstT=dstT: e.activation(out=dstT, in_=pt[:, :], func=AF.Copy), reads=[ptr], awrites=[xT.r])
                else:
                    p.op("dve", lambda e, pt=pt, dstT=dstT: e.tensor_copy(out=dstT, in_=pt[:, :]), reads=[ptr], awrites=[xT.r])
            pl, plr = psb[6], psr[6]

            def fr(e, xT=xT, r=r):
                for c in range(16):
                    q = e.matmul(pl[:, 0:NE], lhsT=xT.t[:, c, :], rhs=wrA[:, r, c, :], start=(c == 0), stop=(c == 15))
                return q
            p.op("pe", fr, reads=[xT.r, wr_r], writes=[plr])
            sm = smR.next()
            p.op("dve", lambda e, sm=sm, ss=ss, r=r: e.scalar_tensor_tensor(out=sm.t[:, 8:8 + NE], in0=pl[:, 0:NE], scalar=ss.t[:, 2:3], in1=biasb[:, r, :],
                                                                         op0=ALU.mult, op1=ALU.add),
                 reads=[plr, ss.r, wr_r], writes=[sm.r])
            p.op("dve", lambda e, sm=sm: e.tensor_reduce(out=sm.t[:, 0:1], in_=sm.t[:, 8:8 + NE], axis=mybir.AxisListType.X, op=ALU.max),
                 reads=[sm.r], writes=[sm.r])
            p.op("dve", lambda e, sm=sm: e.tensor_scalar(out=sm.t[:, 1:2], in0=sm.t[:, 0:1], scalar1=-1.0, scalar2=None, op0=ALU.mult),
                 reads=[sm.r], writes=[sm.r])
            p.op("act", lambda e, sm=sm: e.activation(out=sm.t[:, 8:8 + NE], in_=sm.t[:, 8:8 + NE], func=AF.Exp, bias=sm.t[:, 1:2], accum_out=sm.t[:, 2:3]),
                 reads=[sm.r], writes=[sm.r])
            p.op("dve", lambda e, sm=sm: e.reciprocal(out=sm.t[:, 3:4], in_=sm.t[:, 2:3]), reads=[sm.r], writes=[sm.r])
            p.op("dve", lambda e, sm=sm: e.tensor_scalar(out=sm.t[:, 24:24 + NE], in0=sm.t[:, 8:8 + NE], scalar1=sm.t[:, 3:4], scalar2=None, op0=ALU.mult),
                 reads=[sm.r], writes=[sm.r])
            pa, par = psb[7], psr[7]
            p.op("pe", lambda e, sm=sm: e.transpose(out=pa[0:NE, 0:128], in_=sm.t[:, 24:24 + NE], identity=ident_f[:]), reads=[sm.r, cst_r], writes=[par])
            p.op("dve", lambda e, i=i: e.tensor_copy(out=affT[:, i * 128:(i + 1) * 128], in_=pa[0:NE, 0:128]), reads=[par], awrites=[affT_r])

    def phase5(es, l, need_ctx):
        ncap = CAPT if need_ctx else CAP_L
        chunks = [(0, 128), (128, 128)] + ([(256, 32)] if need_ctx else [])
        Am, Bm, am_r = make_AB(es, l, 2)
        work = AS(es, "tkw", [NE, T], F32)
        mx = AS(es, "tkmx", [NE, CAPT], F32)
        ix = AS(es, "tkix", [NE, CAPT], U32)
        ixf = AS(es, "tkixf", [NE, CAPT], F32)
        tk_r = Res("topk")
        p.op("dve", lambda e: e.tensor_copy(out=work[:], in_=affT[:]), reads=[affT_r], writes=[tk_r])
        segs = [(0, S, 0, CAP_L // 8)] + ([(S, LC, CAP_L, CAP_C // 8)] if need_ctx else [])
        for (c0, cn, o0, rounds) in segs:
            for rd in range(rounds):
                sl = slice(o0 + rd * 8, o0 + rd * 8 + 8)
                p.op("dve", lambda e, sl=sl, c0=c0, cn=cn: e.max(out=mx[:, sl], in_=work[:, c0:c0 + cn]), reads=[tk_r], writes=[tk_r])
                p.op("dve", lambda e, sl=sl, c0=c0, cn=cn: e.max_index(out=ix[:, sl], in_max=mx[:, sl], in_values=work[:, c0:c0 + cn]), reads=[tk_r], writes=[tk_r])
                p.op("dve", lambda e, sl=sl, c0=c0, cn=cn: e.match_replace(out=work[:, c0:c0 + cn], in_to_replace=mx[:, sl], in_values=work[:, c0:c0 + cn], imm_value=-1.0),
                     reads=[tk_r], writes=[tk_r])
        p.op("dve", lambda e: e.tensor_copy(out=ixf[:, 0:ncap], in_=ix[:, 0:ncap]), reads=[tk_r], writes=[tk_r])
        if need_ctx:
            p.op("dve", lambda e: e.tensor_scalar(out=ixf[:, CAP_L:CAPT], in0=ixf[:, CAP_L:CAPT], scalar1=float(S), scalar2=None, op0=ALU.add),
                 reads=[tk_r], writes=[tk_r])
        idxT = AS(es, "idxT", [128, 3, NE], I32)
        gT = AS(es, "gT", [128, 3, NE], F32)
        for j, (a0, an) in enumerate(chunks):
            for src, dstt in ((ixf, idxT), (mx, gT)):
                pa, par = psb[7], psr[7]
                p.op("pe", lambda e, src=src, a0=a0, an=an: e.transpose(out=pa[0:an, 0:NE], in_=src[0:NE, a0:a0 + an], identity=ident_f[0:NE, 0:NE]),
                     reads=[tk_r, cst_r], writes=[par])
                p.op("dve", lambda e, dstt=dstt, j=j, an=an: e.tensor_copy(out=dstt[0:an, j, :], in_=pa[0:an, 0:NE]), reads=[par], awrites=[tk_r])
        g2b = AS(es, "g2b", [128, 2, D], F32)
        g2_r = Res("g2b")
        for r in range(2 if need_ctx else 1):
            p.dma("sp", dma1(g2b[:, r, :], bcast_rows(modrow_d[l, r:r + 1, 5 * D:6 * D], 128)), "sm2", 1, reads=[modrow_r], writes=[g2_r])
        if MOE_STEPS < 2:
            return
        xinR = ringS(es, "mxin", [128, 3, D], BF16, 2)
        xTR = ringS(es, "mxT", [128, 16, CAPT], BF16, 2)
        hidR = ringS(es, "mhid", [128, 8, CAPT], BF16, 2)
        sgR = ringS(es, "msg", [128, CAPT], F32, 2)
        uuR = ringS(es, "muu", [128, CAPT], F32, 2)
        toR = ringS(es, "mto", [128, 512], F32, 2)
        ostR = ringS(es, "most", [128, 3, D], F32, 1)
        psT = PsRing([0, 1])
        psG = PsRing([2, 3])
        psU = PsRing([4, 5])
        psD = PsRing([6, 7])
        wg_all = I("w_gate")[l]
        wu_all = I("w_up")[l]
        wd_all = I("w_down")[l]
        all_x = list(xres_r)

        def gather(e_):
            xin = xinR.next()
            for j, (a0, an) in enumerate(chunks):
                p.dma("pool", lambda e, xin=xin, j=j, an=an, e_=e_: [e.indirect_dma_start(
                    out=xin.t[0:an, j, :], out_offset=None, in_=xn_d[:, :],
                    in_offset=bass.IndirectOffsetOnAxis(ap=idxT[0:an, j, e_:e_ + 1], axis=0))],
                    xin.ds, 1, reads=[xn_r, tk_r], awrites=[xin.r])
            return xin

        def wunit_gu(e_, jb):
            wb = Wr.next()
            wvg = wb.t[:, 0:4096].rearrange("p (c n) -> p c n", c=16)
            wvu = wb.t[:, 4096:8192].rearrange("p (c n) -> p c n", c=16)
            p.dma("pool", dma1(wvg, wg_all[e_, :, jb * 256:(jb + 1) * 256].rearrange("(c k) n -> k c n", k=128)), wb.ds, 1, writes=[wb.r])
            p.dma("pool", dma1(wvu, wu_all[e_, :, jb * 256:(jb + 1) * 256].rearrange("(c k) n -> k c n", k=128)), wb.ds, 1, awrites=[wb.r])
            return wb, wvg, wvu

        def wunit_d(e_, db):
            wb = Wr.next()
            wvd = wb.t[:, 0:4096].rearrange("p (c n) -> p c n", c=8)
            p.dma("pool", dma1(wvd, wd_all[e_, :, db * 512:(db + 1) * 512].rearrange("(c k) n -> k c n", k=128)), wb.ds, 1, writes=[wb.r])
            return wb, wvd

        xin_next = gather(0)
        gu_pre = []
        for e_ in range(NE):
            xin = xin_next
            xT = xTR.next()
            for c in range(16):
                pt, ptr = psT.next()
                ptb = pt[:, :].bitcast(BF16)

                def tr(e, ptb=ptb, xin=xin, c=c):
                    for j, (a0, an) in enumerate(chunks):
                        q = e.transpose(out=ptb[:, a0:a0 + an], in_=xin.t[0:an, j, c * 128:(c + 1) * 128], identity=ident_b[0:an, 0:an])
                    return q
                p.op("pe", tr, reads=[xin.r, cst_r], writes=[ptr])
                p.op("act", lambda e, ptb=ptb, xT=xT, c=c: e.activation(out=xT.t[:, c, 0:CAP_L], in_=ptb[:, 0:CAP_L], func=AF.Identity,
                                                                     scale=Am[:, c, 0:1], bias=Bm[:, c, 0:1]),
                     reads=[ptr, am_r, modcol_r[l]], awrites=[xT.r])
                if need_ctx:
                    p.op("act", lambda e, ptb=ptb, xT=xT, c=c: e.activation(out=xT.t[:, c, CAP_L:CAPT], in_=ptb[:, CAP_L:CAPT], func=AF.Identity,
                                                                         scale=Am[:, c, 1:2], bias=Bm[:, c, 1:2]),
                         reads=[ptr, am_r, modcol_r[l]], awrites=[xT.r])
            if e_ + 1 < NE:
                xin_next = gather(e_ + 1)
            if MOE_STEPS < 3:
                continue
            hid = hidR.next()
            for jb in range(4):
                wb, wvg, wvu = gu_pre.pop(0) if gu_pre else wunit_gu(e_, jb)
                for fc in range(2):
                    pg, pgr = psG.next()
                    pu, pur = psU.next()

                    def fg(e, pg=pg, wvg=wvg, fc=fc, xT=xT):
                        for k in range(16):
                            q = e.matmul(pg[:, 0:ncap], lhsT=wvg[:, k, fc * 128:(fc + 1) * 128], rhs=xT.t[:, k, 0:ncap], start=(k == 0), stop=(k == 15))
                        return q

                    def fu(e, pu=pu, wvu=wvu, fc=fc, xT=xT):
                        for k in range(16):
                            q = e.matmul(pu[:, 0:ncap], lhsT=wvu[:, k, fc * 128:(fc + 1) * 128], rhs=xT.t[:, k, 0:ncap], start=(k == 0), stop=(k == 15))
                        return q
                    p.op("pe", fg, reads=[wb.r, xT.r], writes=[pgr])
                    p.op("pe", fu, reads=[wb.r, xT.r], writes=[pur])
                    sg = sgR.next()
                    p.op("act", lambda e, pg=pg, sg=sg: e.activation(out=sg.t[:, 0:ncap], in_=pg[:, 0:ncap], func=AF.Silu), reads=[pgr], writes=[sg.r])
                    uu = uuR.next()
                    p.op("dve", lambda e, pu=pu, uu=uu: e.tensor_copy(out=uu.t[:, 0:ncap], in_=pu[:, 0:ncap]), reads=[pur], writes=[uu.r])
                    f8 = jb * 2 + fc
                    p.op("dve", lambda e, uu=uu, sg=sg, hid=hid, f8=f8: e.tensor_tensor(out=hid.t[:, f8, 0:ncap], in0=uu.t[:, 0:ncap], in1=sg.t[:, 0:ncap], op=ALU.mult),
                         reads=[uu.r, sg.r], awrites=[hid.r])
            ost = ostR.next()
            for db in range(4):
                wb, wvd = wunit_d(e_, db)
                for j, (a0, an) in enumerate(chunks):
                    r = 0 if j < 2 else 1
                    pd, pdr = psD.next()

                    def fd(e, pd=pd, wvd=wvd, hid=hid, a0=a0, an=an):
                        for f in range(8):
                            q = e.matmul(pd[0:an, :], lhsT=hid.t[:, f, a0:a0 + an], rhs=wvd[:, f, :], start=(f == 0), stop=(f == 7))
                        return q
                    p.op("pe", fd, reads=[wb.r, hid.r], writes=[pdr])
                    to = toR.next()
                    p.op("act", lambda e, pd=pd, to=to, j=j, an=an, e_=e_: e.activation(out=to.t[0:an, :], in_=pd[0:an, :], func=AF.Copy, scale=gT[0:an, j, e_:e_ + 1]),
                         reads=[pdr, tk_r], writes=[to.r])
                    p.op("dve", lambda e, to=to, ost=ost, j=j, an=an, db=db, r=r: e.tensor_tensor(
                        out=ost.t[0:an, j, db * 512:(db + 1) * 512], in0=to.t[0:an, :], in1=g2b[0:an, r, db * 512:(db + 1) * 512], op=ALU.mult),
                        reads=[to.r, g2_r], awrites=[ost.r])
            if e_ + 1 < NE:
                gu_pre.extend(wunit_gu(e_ + 1, jb) for jb in range(2))
            for j, (a0, an) in enumerate(chunks):
                if MOE_STEPS < 4:
                    continue
                p.dma("pool", lambda e, ost=ost, j=j, an=an, e_=e_: [e.indirect_dma_start(
                    out=xres[:, :], out_offset=bass.IndirectOffsetOnAxis(ap=idxT[0:an, j, e_:e_ + 1], axis=0),
                    in_=ost.t[0:an, j, :], in_offset=None, compute_op=ALU.add)],
                    "scat", 1, reads=[ost.r, tk_r], writes=all_x)

    def phase_final(es):
        fnb = AS(es, "fnb", [128, D], F32)
        fn_r = Res("fnb")
        p.dma("sp", dma1(fnb[:], bcast_rows(I("final_norm")[0:1, :], 128)), "sm2", 1, writes=[fn_r])
        xtR = ringS(es, "fx", [128, D], F32, 3)
        oR = ringS(es, "fo", [128, D], F32, 2)
        junk = AS(es, "fjunk", [128, D], BF16)
        junk_r = Res("junkf")
        ssR = ringS(es, "fs", [128, 4], F32, 3)
        for i in range(16):
            xt = xtR.next()
            p.dma("sp", dma1(xt.t[:], xres[i * 128:(i + 1) * 128, :]), xt.ds, 1, reads=[xres_r[i]], writes=[xt.r])
            ss = ssR.next()
            p.op("act", lambda e, xt=xt, ss=ss: e.activation(out=junk[:], in_=xt.t[:], func=AF.Square, accum_out=ss.t[:, 0:1]),
                 reads=[xt.r], writes=[junk_r, ss.r])
            p.op("act", lambda e, ss=ss: e.activation(out=ss.t[:, 1:2], in_=ss.t[:, 0:1], func=AF.Sqrt, scale=1.0 / D, bias=EPS),
                 reads=[ss.r], writes=[ss.r])
            p.op("dve", lambda e, ss=ss: e.reciprocal(out=ss.t[:, 2:3], in_=ss.t[:, 1:2]), reads=[ss.r], writes=[ss.r])
            o = oR.next()
            p.op("dve", lambda e, xt=xt, ss=ss, o=o: e.scalar_tensor_tensor(out=o.t[:], in0=xt.t[:], scalar=ss.t[:, 2:3], in1=fnb[:], op0=ALU.mult, op1=ALU.mult),
                 reads=[xt.r, ss.r, fn_r], writes=[o.r])
            p.dma("sp", dma1(out[i * 128:(i + 1) * 128, :], o.t[:]), o.ds, 1, reads=[o.r], awrites=[out_r])

    for l in range(n_layers):
        is_ab = (l % 2 == 0)
        li = l // 2
        need_ctx = l < DEPTH - 1
        tiles_tok = list(range(NT))
        with ExitStack() as esL:
            big = AS(esL, "big", [128, 16, T], BF16)
            big_rs = [Res(f"big{c}") for c in range(16)]
            with ExitStack() as es1:
                phase1(es1, l, big, big_rs)
                p.barrier()
            if stop == ("p1", l):
                dbg = nc.dram_tensor("dbg_hT", [128, 16, T], BF16, kind="ExternalOutput").ap()
                dr = Res("dbg")
                p.dma("sp", dma1(dbg, big[:]), "dbg", 1, reads=big_rs, writes=[dr])
                return finish([dr])
            with ExitStack() as es2:
                if is_ab:
                    phase2_ab(es2, l, li, big, big_rs, tiles_tok)
                else:
                    phase2_cd(es2, l, li, big, big_rs, tiles_tok)
                p.barrier()
            if stop == ("p2", l):
                return finish()
            with ExitStack() as es3:
                if is_ab:
                    phase3_ab(es3, l, li, big, big_rs, need_ctx)
                else:
                    phase3_cd(es3, l, li, big, big_rs, need_ctx)
                p.barrier()
            if stop == ("p3", l):
                dbg = nc.dram_tensor("dbg_aoT", [128, 16, T], BF16, kind="ExternalOutput").ap()
                dr = Res("dbg")
                p.dma("sp", dma1(dbg, big[:]), "dbg", 1, reads=big_rs, writes=[dr])
                return finish([dr])
            with ExitStack() as es4:
                phase4(es4, l, (I("ab_w_out") if is_ab else I("cd_w_out"))[li], big, big_rs, need_ctx)
                p.barrier()
        if stop == ("p4", l):
            return finish()
        with ExitStack() as es5:
            phase5(es5, l, need_ctx)
            p.barrier()
        if stop == ("p5", l):
            return finish()
    with ExitStack() as esF:
        phase_final(esF)
    return finish()


_CONSTS = None


def _consts():
    global _CONSTS
    if _CONSTS is None:
        (C128, S128, P128), (C64, S64, P64) = ROPE128, ROPE64
        _CONSTS = {
            "c_rc128": _bf(C128), "c_rs128": _bf(S128), "c_rp128": _bf(P128),
            "c_rc64": _bf(C64), "c_rs64": _bf(S64), "c_rp64": _bf(P64),
            "c_band": _bf(BAND), "c_nbrm": _bf(NBR_MASKS.transpose(1, 0, 2)),
            "c_J": np.ascontiguousarray(np.kron(np.eye(2, dtype=np.float32), np.eye(64, dtype=np.float32)[::-1])),
        }
    return _CONSTS


def make_in_maps(inputs, n_cores=N_CORES, used=None):
    f = lambda a: np.ascontiguousarray(np.asarray(a, dtype=np.float32))
    shared = {k: f(inputs[k]) for k in ["w_ada", "b_ada", "norm1", "norm2", "ab_w_in", "ab_sink", "ab_q_norm", "ab_w_uq",
                                        "ab_kv_norm", "ab_w_ukv", "ab_w_out", "cd_w_in", "cd_q_norm", "cd_k_norm", "cd_rpb",
                                        "cd_w_out", "w_router", "w_gate", "w_up", "w_down"]}
    shared["final_norm"] = f(inputs["final_norm"]).reshape(1, D)
    shared.update(_consts())
    x = f(inputs["x"])
    c = f(inputs["c"])
    ctx = f(inputs["ctx"])
    c_ctx = f(inputs["c_ctx"])
    maps = []
    for i in range(n_cores):
        b = i % 4
        m = dict(shared)
        m["x"] = x[b]
        m["ctx"] = ctx[b]
        m["cvec"] = np.stack([c[b], c_ctx], 0)
        if used is not None:
            m = {k: v for k, v in m.items() if k in used}
        maps.append(m)
    return maps


def kernel(**inputs):
    nc = build()
    maps = make_in_maps(inputs, used=nc.used_inputs)
    res = run_bass_kernel_spmd(nc, maps, core_ids=list(range(N_CORES)))
    return np.stack([res.results[b]["out"] for b in range(4)], 0).astype(np.float32)
```
